# Optimizing a Trainium2 kernel written in Bass

```python
import jax
import jax.numpy as jnp
from jax import lax
import numpy as np

D_MODEL = 1024
BATCH = 16
SEQ = 2048
DEPTH = 1

CTX_LEN = 256
GRID_W = 64
N_HEADS = 16
N_KV_HEADS = 4
GROUP = N_HEADS // N_KV_HEADS
HEAD_DIM = D_MODEL // N_HEADS
ATTN_WIDTH = N_HEADS * HEAD_DIM
KV_WIDTH = N_KV_HEADS * HEAD_DIM
WINDOW = 128
Q_BLOCK = 128
ROPE_BASE = 10000.0
CONV_WIDTH = D_MODEL
CONV_SIZE = 3
N_EXPERTS = 16
EXPERT_FF = D_MODEL
CAPACITY_FACTOR = 2
N_MOD = 6
IN_WIDTH = ATTN_WIDTH + 2 * KV_WIDTH + 3 * CONV_WIDTH + 2 * D_MODEL
EPS = 1e-6
NEG_INF = -1e30

kernel_name = "hybrid_gated_swa_shortconv_ecmoe_dit"


def rmsnorm(x, g):
    x32 = x.astype(jnp.float32)
    y = x32 * lax.rsqrt(jnp.mean(x32 * x32, axis=-1, keepdims=True) + EPS)
    return (y * g.astype(jnp.float32)).astype(x.dtype)


def modulate(h, shift, scale):
    return h * (1 + scale) + shift


def split_projection(p):
    sizes = (ATTN_WIDTH, KV_WIDTH, KV_WIDTH, CONV_WIDTH, CONV_WIDTH, CONV_WIDTH, D_MODEL, D_MODEL)
    parts = []
    off = 0
    for s in sizes:
        parts.append(p[..., off:off + s])
        off += s
    return parts


def axial_rope_tables(n_tokens, dtype):
    rows = n_tokens // GRID_W
    row = jnp.repeat(jnp.arange(rows, dtype=jnp.float32), GRID_W)
    col = jnp.tile(jnp.arange(GRID_W, dtype=jnp.float32), rows)
    n_freq = HEAD_DIM // 4
    inv_freq = ROPE_BASE ** (-jnp.arange(n_freq, dtype=jnp.float32) / n_freq)
    ang_r = row[:, None] * inv_freq[None, :]
    ang_c = col[:, None] * inv_freq[None, :]
    ang = jnp.concatenate([ang_r, ang_r, ang_c, ang_c], axis=-1)[:, None, :]
    return jnp.cos(ang).astype(dtype), jnp.sin(ang).astype(dtype)


def rotate_half(z):
    z1, z2 = jnp.split(z, 2, axis=-1)
    return jnp.concatenate([-z2, z1], axis=-1)


def apply_axial_rope(x, cos, sin):
    xr, xc = jnp.split(x, 2, axis=-1)
    rot = jnp.concatenate([rotate_half(xr), rotate_half(xc)], axis=-1)
    return x * cos + rot * sin


def sink_column(sink, shape_prefix):
    return jnp.broadcast_to(sink.astype(jnp.float32)[None, :, :, None, None], shape_prefix + (1,))


def latent_window_attention(q, k, v, k_ctx, v_ctx, sink):
    B, T = q.shape[0], q.shape[1]
    L = k_ctx.shape[1]
    n_blocks = T // Q_BLOCK
    span = Q_BLOCK + 2 * WINDOW
    scale = HEAD_DIM ** -0.5
    pad = ((0, 0), (WINDOW, WINDOW), (0, 0), (0, 0))
    kp = jnp.pad(k, pad)
    vp = jnp.pad(v, pad)
    key_offset = jnp.arange(span) - WINDOW

    def block(i):
        start = i * Q_BLOCK
        qb = lax.dynamic_slice_in_dim(q, start, Q_BLOCK, axis=1)
        kb = lax.dynamic_slice_in_dim(kp, start, span, axis=1)
        vb = lax.dynamic_slice_in_dim(vp, start, span, axis=1)
        s_lat = jnp.einsum('bqhgd,bshd->bhgqs', qb, kb).astype(jnp.float32) * scale
        s_ctx = jnp.einsum('bqhgd,bshd->bhgqs', qb, k_ctx).astype(jnp.float32) * scale
        qpos = start + jnp.arange(Q_BLOCK)
        kpos = start + key_offset
        valid = (jnp.abs(qpos[:, None] - kpos[None, :]) <= WINDOW) & (kpos >= 0)[None, :] & (kpos < T)[None, :]
        s_lat = jnp.where(valid, s_lat, NEG_INF)
        logits = jnp.concatenate([s_lat, s_ctx, sink_column(sink, s_lat.shape[:-1])], axis=-1)
        p = jax.nn.softmax(logits, axis=-1)
        p_lat = p[..., :span].astype(v.dtype)
        p_ctx = p[..., span:span + L].astype(v.dtype)
        return (jnp.einsum('bhgqs,bshd->bqhgd', p_lat, vb)
                + jnp.einsum('bhgqs,bshd->bqhgd', p_ctx, v_ctx))

    o = lax.map(block, jnp.arange(n_blocks))
    return jnp.transpose(o, (1, 0, 2, 3, 4, 5)).reshape(B, T, ATTN_WIDTH)


def context_attention(q, k, v, sink):
    B, L = q.shape[0], q.shape[1]
    scale = HEAD_DIM ** -0.5
    s = jnp.einsum('bqhgd,bshd->bhgqs', q, k).astype(jnp.float32) * scale
    p = jax.nn.softmax(jnp.concatenate([s, sink_column(sink, s.shape[:-1])], axis=-1), axis=-1)
    o = jnp.einsum('bhgqs,bshd->bqhgd', p[..., :L].astype(v.dtype), v)
    return o.reshape(B, L, ATTN_WIDTH)


def short_conv_mixer(u, gate_b, gate_c, conv_w):
    z = gate_c * u
    zp = jnp.pad(z, ((0, 0), (1, 1), (0, 0)))
    y = conv_w[0] * zp[:, :-2] + conv_w[1] * zp[:, 1:-1] + conv_w[2] * zp[:, 2:]
    return gate_b * y


def gated_merge(o_attn, y_conv, g_attn, g_conv, w_proj_attn, w_proj_conv, w_out):
    a = jnp.einsum('btf,fd->btd', o_attn, w_proj_attn)
    s = jnp.einsum('btf,fd->btd', y_conv, w_proj_conv)
    merged = jax.nn.sigmoid(g_attn) * a + jax.nn.sigmoid(g_conv) * s
    return jnp.einsum('btd,de->bte', merged, w_out)


def expert_choice_moe(h, w_router, w_gate, w_up, w_down):
    B, T, D = h.shape
    cap = CAPACITY_FACTOR * T // N_EXPERTS
    logits = jnp.einsum('btd,de->bte', h, w_router).astype(jnp.float32)
    affinity = jax.nn.softmax(logits, axis=-1)
    gate, idx = lax.top_k(jnp.swapaxes(affinity, 1, 2), cap)
    xe = jax.vmap(lambda hb, ib: hb[ib])(h, idx)
    a = jnp.einsum('becd,edf->becf', xe, w_gate)
    u = jnp.einsum('becd,edf->becf', xe, w_up)
    ye = jnp.einsum('becf,efd->becd', jax.nn.silu(a) * u, w_down) * gate.astype(h.dtype)[..., None]

    def combine(yb, ib):
        return jnp.zeros((T, D), yb.dtype).at[ib.reshape(-1)].add(yb.reshape(-1, D))

    return jax.vmap(combine)(ye, idx)


def setup_inputs(seed: int = 0) -> dict:
    key = jax.random.key(seed)
    ks = jax.random.split(key, 20)
    f32 = jnp.float32

    def nrm(k, shape, scale):
        return jax.random.normal(k, shape, f32) * scale

    return {
        'x': nrm(ks[0], (BATCH, SEQ, D_MODEL), 1.0),
        'c': nrm(ks[1], (BATCH, D_MODEL), 1.0),
        'ctx': nrm(ks[2], (BATCH, CTX_LEN, D_MODEL), 1.0),
        'c_ctx': nrm(ks[3], (D_MODEL,), 1.0),
        'w_mod': nrm(ks[4], (DEPTH, D_MODEL, N_MOD * D_MODEL), 0.5 * D_MODEL ** -0.5),
        'b_mod': nrm(ks[5], (DEPTH, N_MOD * D_MODEL), 0.02),
        'norm1_g': 1.0 + nrm(ks[6], (DEPTH, D_MODEL), 0.02),
        'w_in': nrm(ks[7], (DEPTH, D_MODEL, IN_WIDTH), D_MODEL ** -0.5),
        'attn_sink': nrm(ks[8], (DEPTH, N_KV_HEADS, GROUP), 0.5),
        'conv_w': nrm(ks[9], (DEPTH, CONV_SIZE, CONV_WIDTH), CONV_SIZE ** -0.5),
        'w_proj_attn': nrm(ks[10], (DEPTH, ATTN_WIDTH, D_MODEL), ATTN_WIDTH ** -0.5),
        'w_proj_conv': nrm(ks[11], (DEPTH, CONV_WIDTH, D_MODEL), CONV_WIDTH ** -0.5),
        'w_out': nrm(ks[12], (DEPTH, D_MODEL, D_MODEL), D_MODEL ** -0.5),
        'norm2_g': 1.0 + nrm(ks[13], (DEPTH, D_MODEL), 0.02),
        'w_router': nrm(ks[14], (DEPTH, D_MODEL, N_EXPERTS), D_MODEL ** -0.5),
        'w_exp_gate': nrm(ks[15], (DEPTH, N_EXPERTS, D_MODEL, EXPERT_FF), D_MODEL ** -0.5),
        'w_exp_up': nrm(ks[16], (DEPTH, N_EXPERTS, D_MODEL, EXPERT_FF), D_MODEL ** -0.5),
        'w_exp_down': nrm(ks[17], (DEPTH, N_EXPERTS, EXPERT_FF, D_MODEL), EXPERT_FF ** -0.5),
        'final_norm_g': 1.0 + nrm(ks[18], (D_MODEL,), 0.02),
    }


def reference(x, c, ctx, c_ctx, w_mod, b_mod, norm1_g, w_in, attn_sink, conv_w,
              w_proj_attn, w_proj_conv, w_out, norm2_g, w_router, w_exp_gate, w_exp_up,
              w_exp_down, final_norm_g):
    B, T = x.shape[0], x.shape[1]
    L = ctx.shape[1]
    cos, sin = axial_rope_tables(T, x.dtype)
    h_ctx_stream = ctx
    for layer in range(DEPTH):
        last = layer == DEPTH - 1
        mod = jnp.einsum('bd,de->be', jax.nn.silu(c), w_mod[layer]) + b_mod[layer]
        sh1, sc1, g1, sh2, sc2, g2 = jnp.split(mod[:, None, :], N_MOD, axis=-1)
        mod_c = jnp.einsum('d,de->e', jax.nn.silu(c_ctx), w_mod[layer]) + b_mod[layer]
        csh1, csc1, cg1, csh2, csc2, cg2 = jnp.split(mod_c, N_MOD, axis=-1)

        hc = modulate(rmsnorm(h_ctx_stream, norm1_g[layer]), csh1, csc1)
        if last:
            kv_c = jnp.einsum('btd,df->btf', hc, w_in[layer][:, ATTN_WIDTH:ATTN_WIDTH + 2 * KV_WIDTH])
            k_c, v_c = jnp.split(kv_c, 2, axis=-1)
        else:
            pc = split_projection(jnp.einsum('btd,df->btf', hc, w_in[layer]))
            q_c, k_c, v_c, u_c, b_c, c_c, ga_c, gc_c = pc
        k_ctx = k_c.reshape(B, L, N_KV_HEADS, HEAD_DIM)
        v_ctx = v_c.reshape(B, L, N_KV_HEADS, HEAD_DIM)

        hx = modulate(rmsnorm(x, norm1_g[layer]), sh1, sc1)
        q_x, k_x, v_x, u_x, b_x, c_x, ga_x, gc_x = split_projection(
            jnp.einsum('btd,df->btf', hx, w_in[layer]))
        q = apply_axial_rope(q_x.reshape(B, T, N_HEADS, HEAD_DIM), cos, sin)
        q = q.reshape(B, T, N_KV_HEADS, GROUP, HEAD_DIM)
        k = apply_axial_rope(k_x.reshape(B, T, N_KV_HEADS, HEAD_DIM), cos, sin)
        v = v_x.reshape(B, T, N_KV_HEADS, HEAD_DIM)
        o_attn = latent_window_attention(q, k, v, k_ctx, v_ctx, attn_sink[layer])
        y_conv = short_conv_mixer(u_x, b_x, c_x, conv_w[layer])
        x = x + g1 * gated_merge(o_attn, y_conv, ga_x, gc_x,
                                 w_proj_attn[layer], w_proj_conv[layer], w_out[layer])

        hx2 = modulate(rmsnorm(x, norm2_g[layer]), sh2, sc2)
        x = x + g2 * expert_choice_moe(hx2, w_router[layer], w_exp_gate[layer],
                                       w_exp_up[layer], w_exp_down[layer])

        if not last:
            o_c = context_attention(q_c.reshape(B, L, N_KV_HEADS, GROUP, HEAD_DIM),
                                    k_ctx, v_ctx, attn_sink[layer])
            y_c = short_conv_mixer(u_c, b_c, c_c, conv_w[layer])
            h_ctx_stream = h_ctx_stream + cg1 * gated_merge(
                o_c, y_c, ga_c, gc_c, w_proj_attn[layer], w_proj_conv[layer], w_out[layer])
            hc2 = modulate(rmsnorm(h_ctx_stream, norm2_g[layer]), csh2, csc2)
            h_ctx_stream = h_ctx_stream + cg2 * expert_choice_moe(
                hc2, w_router[layer], w_exp_gate[layer], w_exp_up[layer], w_exp_down[layer])

    return rmsnorm(x, final_norm_g)
```

```python
import os
import numpy as np
import ml_dtypes
from contextlib import ExitStack
import concourse.bass as bass
import concourse.mybir as mybir
from concourse.bass_utils import run_bass_kernel_spmd

F32 = mybir.dt.float32
BF16 = mybir.dt.bfloat16
AF = mybir.ActivationFunctionType
ALU = mybir.AluOpType

D = 1024
T = 2048
L = 256
NS = 2
NE = 16
CAP = 256
EPS = 1e-6
IN_W = 6656
OFF_Q, OFF_K, OFF_V, OFF_U, OFF_B, OFF_C, OFF_GA, OFF_GC = 0, 1024, 1280, 1536, 2560, 3584, 4608, 5632

SEM_CH = 4000
STQ = "pool"
DMA_RING = 8


class Reg:
    __slots__ = ("name", "lw", "rd")

    def __init__(self, name=""):
        self.name = name
        self.lw = {}
        self.rd = []


class Ev:
    __slots__ = ("key", "seq", "sem", "val")

    def __init__(self, key, seq, sem, val):
        self.key, self.seq, self.sem, self.val = key, seq, sem, val


class Sched:
    def __init__(self, nc, stack):
        self.nc = nc
        self.stack = stack
        self.eng = {"pe": nc.tensor, "act": nc.scalar, "dve": nc.vector, "pool": nc.gpsimd, "sp": nc.sync}
        self.cnt = {k: 0 for k in self.eng}
        self.sems = {k: [] for k in self.eng}
        self.last = {k: None for k in self.eng}
        self.waited = {k: {} for k in self.eng}
        self.dq = {}
        self.same_engine_sync = {"act": True, "dve": True, "pool": True, "pe": False, "sp": False}
        self.n_wait = 0
        self.n_inst = 0
        self.marks = []
        self.marker = None

    def _sem(self, name):
        return self.stack.enter_context(self.nc.semaphore(name))

    def _eng_event(self, e):
        seq = self.cnt[e]
        self.cnt[e] += 1
        ci = seq // SEM_CH
        while len(self.sems[e]) <= ci:
            self.sems[e].append(self._sem(f"s_{e}_{len(self.sems[e])}"))
        ev = Ev(e, seq, self.sems[e][ci], seq % SEM_CH + 1)
        self.last[e] = ev
        return ev

    def _wait(self, e, ev):
        if ev is None:
            return
        if ev.key == e and not self.same_engine_sync[e]:
            return
        w = self.waited[e]
        if w.get(ev.key, -1) >= ev.seq:
            return
        self.eng[e].wait_ge(ev.sem, ev.val)
        self.n_wait += 1
        w[ev.key] = ev.seq

    def _deps(self, e, reads, writes, nowaw=False):
        need = {}

        def add(ev):
            o = need.get(ev.key)
            if o is None or o.seq < ev.seq:
                need[ev.key] = ev

        for r in reads:
            for ev in r.lw.values():
                add(ev)
        for wr in writes:
            if not nowaw:
                for ev in wr.lw.values():
                    add(ev)
            for ev in wr.rd:
                add(ev)
        for ev in need.values():
            self._wait(e, ev)

    def _commit(self, ev, reads, writes, nowaw=False):
        for r in reads:
            r.rd.append(ev)
            if len(r.rd) > 48:
                d = {}
                for x in r.rd:
                    if x.key not in d or d[x.key].seq < x.seq:
                        d[x.key] = x
                r.rd = list(d.values())
        for wr in writes:
            if nowaw:
                wr.lw[ev.key] = ev
            else:
                wr.lw = {ev.key: ev}
            wr.rd = []

    def op(self, e, fn, reads=(), writes=(), nowaw=False):
        self._deps(e, reads, writes, nowaw)
        ev = self._eng_event(e)
        fn(self.eng[e]).then_inc(ev.sem, 1)
        self.n_inst += 1
        self._commit(ev, reads, writes, nowaw)
        return ev

    def dma(self, q, out, in_, reads=(), writes=(), nowaw=False, **kw):
        st = self.dq.setdefault(q, {"n": 0, "sems": [], "evs": {}})
        n = st["n"]
        st["n"] += 1
        slot = n % DMA_RING
        while len(st["sems"]) <= slot:
            st["sems"].append(self._sem(f"d_{q}_{len(st['sems'])}"))
        self._wait(q, st["evs"].get(slot))
        self._deps(q, reads, writes, nowaw)
        ev = Ev(f"dma_{q}_{slot}", n // DMA_RING, st["sems"][slot], 16 * (n // DMA_RING + 1))
        self.eng[q].dma_start(out=out, in_=in_, **kw).then_inc(ev.sem, 16)
        st["evs"][slot] = ev
        self.n_inst += 1
        self._commit(ev, reads, writes, nowaw)
        return ev

    def dma_custom(self, q, issue, reads=(), writes=(), nowaw=False):
        st = self.dq.setdefault(q, {"n": 0, "sems": [], "evs": {}})
        n = st["n"]
        st["n"] += 1
        slot = n % DMA_RING
        while len(st["sems"]) <= slot:
            st["sems"].append(self._sem(f"d_{q}_{len(st['sems'])}"))
        self._wait(q, st["evs"].get(slot))
        self._deps(q, reads, writes, nowaw)
        ev = Ev(f"dma_{q}_{slot}", n // DMA_RING, st["sems"][slot], 16 * (n // DMA_RING + 1))
        issue(self.eng[q]).then_inc(ev.sem, 16)
        st["evs"][slot] = ev
        self.n_inst += 1
        self._commit(ev, reads, writes, nowaw)
        return ev

    def barrier(self):
        import sys as _sys
        self.marks.append((_sys._getframe(1).f_lineno, dict(self.cnt)))
        evs = [ev for ev in self.last.values() if ev is not None]
        for st in self.dq.values():
            evs.extend(st["evs"].values())
        for e in self.eng:
            for ev in evs:
                if ev.key != e:
                    self._wait(e, ev)
        if self.marker is not None:
            self.eng["pool"].memset(self.marker, float(len(self.marks)))

    def finish(self):
        evs = [ev for ev in self.last.values() if ev is not None]
        for st in self.dq.values():
            evs.extend(st["evs"].values())
        for ev in evs:
            if ev.key != "sp":
                self._wait("sp", ev)


def build_program(dbg=None, stop_after=None, skip_pre=False):
    nc = bass.Bass("TRN2", target_bir_lowering=False)

    def din(name, shape, dt=F32):
        return nc.dram_tensor(name, list(shape), dt, kind="ExternalInput").ap()

    def dscr(name, shape, dt):
        return nc.dram_tensor(name, list(shape), dt, kind="Internal").ap()

    x_d = din("x", [NS, T, D])
    ctx_d = din("ctx", [NS, L, D])
    cvec_d = din("cvec", [128, 8, 4])
    wmod_d = din("w_mod", [D, 6 * D])
    bmod_d = din("b_mod", [1, 6 * D])
    bmodT_d = din("bmodT", [128, 16])
    n1gT_d = din("n1gT", [128, 8])
    n2g_d = din("n2g", [1, D])
    fng_d = din("fng", [1, D])
    win_d = din("w_in", [D, IN_W])
    sink_d = din("sink", [1, 16])
    convw_d = din("convwT", [128, 8, 3])
    wpa_d = din("w_pa", [D, D])
    wpc_d = din("w_pc", [D, D])
    wout_d = din("w_out", [D, D])
    wr_d = din("w_r", [D, NE])
    weg_d = din("w_eg", [NE, D, D])
    weu_d = din("w_eu", [NE, D, D])
    wed_d = din("w_ed", [NE, D, D])
    cosT_d = din("cosT", [128, T])
    sinT_d = din("sinT", [128, T])
    rmat_d = din("rmat", [128, 128], BF16)
    mle_d = din("mask_le", [128, 128], BF16)
    mge_d = din("mask_ge", [128, 128], BF16)
    identb_d = din("ident_bf", [128, 128], BF16)
    identf_d = din("ident_f", [128, 128])
    iota_d = din("iota256", [128, 256])
    iotap_d = din("iota_p", [128, 2])
    tokhl_d = din("tokhl", [128, 16, 2], BF16)
    out_d = nc.dram_tensor("out", [NS, T, D], F32, kind="ExternalOutput").ap()
    dbg_d = {}
    if dbg:
        for name, shape in dbg.items():
            dbg_d[name] = nc.dram_tensor("dbg_" + name, list(shape), F32, kind="ExternalOutput").ap()

    modrows_d = dscr("modrows", [4, 6 * D], F32)
    md_d = dscr("md", [NS, 8, 128, T], BF16)
    x1_d = dscr("x1", [NS, T, D], F32)
    hx2_d = dscr("hx2", [NS, T, D], BF16)
    posr_d = dscr("posr", [NS, NE, T], BF16)
    ye_d = dscr("ye", [NS, NE, CAP, D], BF16)

    with ExitStack() as top:
        S = Sched(nc, top)

        uid = [0]

        def sb(st, name, shape, dt):
            uid[0] += 1
            return st.enter_context(nc.sbuf_tensor(f"sb{uid[0]}_{name}", list(shape), dt))

        def ps(st, name, shape, dt=F32):
            uid[0] += 1
            return st.enter_context(nc.psum_tensor(f"ps{uid[0]}_{name}", list(shape), dt))

        def mm(out, lhsT, rhs, start, stop, reads, writes):
            S.op("pe", lambda e: e.matmul(out, lhsT=lhsT, rhs=rhs, start=start, stop=stop), reads, writes)

        def tr(out, in_, ident, reads, writes):
            S.op("pe", lambda e: e.transpose(out, in_, ident), reads, writes)

        def act(out, in_, func, reads, writes, **kw):
            S.op("act", lambda e: e.activation(out=out, in_=in_, func=func, **kw), reads, writes)

        def tt(eng, out, in0, in1, op, reads, writes):
            S.op(eng, lambda e: e.tensor_tensor(out=out, in0=in0, in1=in1, op=op), reads, writes)

        def ts(eng, out, in0, s1, s2, op0, op1, reads, writes):
            if op1 is None:
                S.op(eng, lambda e: e.tensor_scalar(out=out, in0=in0, scalar1=s1, scalar2=None, op0=op0), reads, writes)
            else:
                S.op(eng, lambda e: e.tensor_scalar(out=out, in0=in0, scalar1=s1, scalar2=s2, op0=op0, op1=op1), reads, writes)

        def stt(out, in0, scalar, in1, op0, op1, reads, writes):
            S.op("dve", lambda e: e.scalar_tensor_tensor(out=out, in0=in0, scalar=scalar, in1=in1, op0=op0, op1=op1), reads, writes)

        def cp(eng, out, in_, reads, writes, nowaw=False):
            if eng == "act":
                S.op("act", lambda e: e.activation(out=out, in_=in_, func=AF.Copy), reads, writes, nowaw)
            else:
                S.op(eng, lambda e: e.tensor_copy(out=out, in_=in_), reads, writes, nowaw)

        def pipeline(n, stages, skew=1):
            for step in range(n + (len(stages) - 1) * skew):
                for k, f in enumerate(stages):
                    i = step - k * skew
                    if 0 <= i < n:
                        f(i)

        def dbg_dump(name, src_ap, reads, view=None):
            if name in dbg_d:
                dst = dbg_d[name] if view is None else view(dbg_d[name])
                S.dma("pool", dst, src_ap, reads=reads, writes=[Reg()])

        identb = sb(top, "identb", [128, 128], BF16)
        identf = sb(top, "identf", [128, 128], F32)
        rmat = sb(top, "rmat", [128, 128], BF16)
        mle = sb(top, "mle", [128, 128], BF16)
        mge = sb(top, "mge", [128, 128], BF16)
        iota = sb(top, "iota", [128, 256], F32)
        iotap = sb(top, "iotap", [128, 2], F32)
        convw = sb(top, "convw", [128, 8, 3], F32)
        n1gT = sb(top, "n1gT_s", [128, 8], F32)
        bmodT = sb(top, "bmodT_s", [128, 16], F32)
        esink = sb(top, "esink", [128, 16], F32)
        modT = sb(top, "modT", [128, 16, 4], F32)
        A1s = sb(top, "A1s", [128, 8, 4], F32)
        wr_f = sb(top, "wr_f", [128, 8, 128], F32)
        aff = sb(top, "aff", [64, T], F32)
        ones16 = sb(top, "ones16", [64, 64], F32)
        epsc = sb(top, "epsc", [128, 1], F32)
        R_c = Reg("consts")
        if os.environ.get("MARKS"):
            mark_t = sb(top, "mark_t", [1, 8], F32)
            S.marker = mark_t[0:1, 0:1]
        for dst, src in ((identb, identb_d), (identf, identf_d), (rmat, rmat_d), (mle, mle_d), (mge, mge_d),
                         (iota, iota_d), (iotap, iotap_d), (convw, convw_d), (n1gT, n1gT_d), (bmodT, bmodT_d)):
            S.dma("sp", dst[:], src, writes=[R_c], nowaw=True)
        S.dma("sp", esink[:], sink_d.to_broadcast([128, 16]), writes=[R_c], nowaw=True)
        S.op("dve", lambda e: e.memset(wr_f[:], 0.0), writes=[R_c])
        S.op("dve", lambda e: e.memset(aff[:], 0.0), writes=[R_c])
        S.op("dve", lambda e: e.memset(ones16[:], 0.0), writes=[R_c])
        S.op("dve", lambda e: e.memset(epsc[:], EPS), writes=[R_c])
        S.op("dve", lambda e: e.memset(ones16[0:16, 0:16], 1.0), writes=[R_c])
        S.op("dve", lambda e: e.memset(ones16[32:48, 32:48], 1.0), writes=[R_c])
        wrv = wr_d.rearrange("(k p) e -> p k e", p=128)
        S.dma("sp", wr_f[:, :, 0:16], wrv, writes=[R_c])
        S.dma("sp", wr_f[:, :, 32:48], wrv, writes=[R_c])
        act(esink[:], esink[:], AF.Exp, [R_c], [R_c])
        S.barrier()

        R_modT = Reg()
        R_modrows = Reg()
        with ExitStack() as st:
            cv = sb(st, "cv", [128, 8, 4], F32)
            scv = sb(st, "scv", [128, 8, 4], F32)
            bm3 = sb(st, "bm3", [4, 6 * D], F32)
            wmb = [sb(st, f"wmb{i}", [128, 8, 512], F32) for i in range(3)]
            mrow = [sb(st, f"mrow{i}", [4, 512], F32) for i in range(4)]
            tmp1 = sb(st, "tmp1", [128, 8, 4], F32)
            ps_r = [ps(st, f"ps_r{i}", [128, 512]) for i in range(2)]
            ps_f = [ps(st, f"ps_f{i}", [128, 512]) for i in range(2)]
            R_cv, R_bm3 = Reg(), Reg()
            R_wmb = [Reg(), Reg(), Reg()]
            R_mrow = [Reg() for _ in range(4)]
            R_psr = [Reg(), Reg()]
            R_psf = [Reg(), Reg()]
            R_modT = Reg()
            R_modrows = Reg()
            S.dma("sp", cv[:], cvec_d, writes=[R_cv])
            S.dma("sp", bm3[:], bmod_d.to_broadcast([4, 6 * D]), writes=[R_bm3])
            act(scv[:], cv[:], AF.Silu, [R_cv], [R_cv])
            wmv = wmod_d.rearrange("(k p) n -> p k n", p=128)
            nf = 0
            for b in range(12):
                w = wmb[b % 3]
                Rw = R_wmb[b % 3]
                S.dma("sp", w[:], wmv[:, :, b * 512:(b + 1) * 512], writes=[Rw])
                pr = ps_r[b % 2]
                mr, Rm = mrow[b % 4], R_mrow[b % 4]
                for k in range(8):
                    mm(pr[0:4, :], scv[:, k, :], w[:, k, :], k == 0, k == 7, [R_cv, Rw], [R_psr[b % 2]])
                tt("dve", mr[:], pr[0:4, :], bm3[:, b * 512:(b + 1) * 512], ALU.add, [R_psr[b % 2], R_bm3], [Rm])
                if b >= 4:
                    S.dma("sp", modrows_d[:, b * 512:(b + 1) * 512], mr[:], reads=[Rm], writes=[R_modrows], nowaw=True)
                else:
                    pf = ps_f[b % 2]
                    for j in range(4):
                        tr(pf[:, j * 4:(j + 1) * 4], mr[0:4, j * 128:(j + 1) * 128], identf[0:4, 0:4], [Rm, R_c], [R_psf[b % 2]])
                    cp("dve", modT[:, b * 4:(b + 1) * 4, :], pf[:, 0:16].rearrange("p (j v) -> p j v", v=4), [R_psf[b % 2]], [R_modT])
            ts("dve", tmp1[:], modT[:, 8:16, :], 1.0, None, ALU.add, None, [R_modT], [R_cv])
            for j in range(4):
                tt("dve", A1s[:, :, j], tmp1[:, :, j], n1gT[:], ALU.mult, [R_cv, R_c], [R_modT])
            S.barrier()
        if dbg:
            dbg_dump("modT", modT[:], [R_modT])
            dbg_dump("A1s", A1s[:], [R_modT])

        if stop_after == "P0":
            S.barrier(); S.finish(); return nc

        cast_rr = [0]

        def load_w(st_tiles, R_st, dst_ap, pieces, R_dst, ncols):
            i = cast_rr[0]
            cast_rr[0] += 1
            stg = st_tiles[i % len(st_tiles)]
            Rs = R_st[i % len(st_tiles)]
            for (c0, src, wd) in pieces:
                S.dma("sp", stg[:, :, c0:c0 + wd], src, writes=[Rs], nowaw=True)
            eng = ("act", "act", "dve")[i % 3]
            cp(eng, dst_ap, stg[:, :, 0:ncols], [Rs], [R_dst], nowaw=True)

        winv = win_d.rearrange("(k p) n -> p k n", p=128)

        def rms_rstd(st_name, xt, R_x, junk, R_junk, ssq, rstd, R_stat, recip=True):
            act(junk, xt, AF.Square, [R_x], [R_junk, R_stat], accum_out=ssq)
            act(ssq, ssq, AF.Sqrt, [R_stat], [R_stat], scale=1.0 / D, bias=epsc[:, 0:1])
            if recip:
                S.op("dve", lambda e: e.reciprocal(out=rstd, in_=ssq), [R_stat], [R_stat])

        R_aff = Reg("aff")
        R_x1d, R_hx2d = Reg(), Reg()

        for s in range(0 if skip_pre else NS):
            with ExitStack() as sa:
                H = sb(sa, "H", [128, 8, T], BF16)
                R_H = [[Reg() for _ in range(4)] for _ in range(8)]
                QO = sb(sa, "QO", [128, 8, T], BF16)
                R_Q = [[Reg() for _ in range(16)] for _ in range(8)]
                stg = [sb(sa, f"stg{i}", [128, 8, 256], F32) for i in range(3)]
                R_stg = [Reg() for _ in range(3)]
                wring = [sb(sa, f"wring{i}", [128, 8, 256], BF16) for i in range(4)]
                R_wr = [Reg() for _ in range(4)]
                wi = [0]

                def next_w(pieces, ncols=256, ring=None):
                    tiles, regs, cnt = ring if ring is not None else (wring, R_wr, wi)
                    i = cnt[0] % len(tiles)
                    cnt[0] += 1
                    load_w(stg, R_stg, tiles[i][:, :, 0:ncols], pieces, regs[i], ncols)
                    return tiles[i], regs[i]

                with ExitStack() as st:
                    KtA = sb(st, "KtA", [128, 4, T + L], BF16)
                    KtB = sb(st, "KtB", [128, 4, T + L], BF16)
                    Kt2 = [KtA, KtB]
                    R_K = [[Reg() for _ in range(18)] for _ in range(4)]
                    S.op("pool", lambda e: e.memset(KtA[64:128, :, :], 0.0), writes=[r for rr in R_K for r in rr])
                    S.op("pool", lambda e: e.memset(KtB[0:64, :, :], 0.0), writes=[r for rr in R_K for r in rr], nowaw=True)
                    V = sb(st, "V", [128, 18, 4, 65], BF16)
                    R_V = [Reg() for _ in range(18)]
                    Hc = sb(st, "Hc", [128, 8, L], BF16)
                    R_Hc = Reg()
                    R_cs = Reg()
                    S.op("pool", lambda e: e.memset(V[:, :, :, 64:65], 1.0), writes=R_V)

                    with ExitStack() as s1:
                        xt = [sb(s1, f"xt{i}", [128, D], F32) for i in range(2)]
                        xn = [sb(s1, f"xn{i}", [128, D], F32) for i in range(2)]
                        junk = sb(s1, "junk1", [128, D], F32)
                        stat = [sb(s1, f"stat{i}", [128, 2], F32) for i in range(2)]
                        pst = [ps(s1, f"pst{i}", [128, 8, 128]) for i in range(2)]
                        R_xt, R_xn, R_st1 = [Reg(), Reg()], [Reg(), Reg()], [Reg(), Reg()]
                        R_pst = [[Reg(), Reg()], [Reg(), Reg()]]
                        R_junk = Reg()
                        def a1_L(t):
                            b = t % 2
                            src = x_d[s, t * 128:(t + 1) * 128, :] if t < 16 else ctx_d[s, (t - 16) * 128:(t - 15) * 128, :]
                            S.dma("sp", xt[b][:], src, writes=[R_xt[b]])
                            rms_rstd("a1", xt[b][:], R_xt[b], junk[:], R_junk, stat[b][:, 0:1], stat[b][:, 1:2], R_st1[b])
                            ts("dve", xn[b][:], xt[b][:], stat[b][:, 1:2], None, ALU.mult, None, [R_xt[b], R_st1[b]], [R_xn[b]])

                        def a1_X(t):
                            b = t % 2
                            mj = s if t < 16 else 2
                            for k in range(8):
                                tr(pst[b][:, k, :], xn[b][:, k * 128:(k + 1) * 128], identf[:], [R_xn[b], R_c], [R_pst[b][k // 4]])
                            for k in range(8):
                                if t < 16:
                                    dst = H[:, k, t * 128:(t + 1) * 128]
                                    Rd = [R_H[k][t // 4]]
                                else:
                                    dst = Hc[:, k, (t - 16) * 128:(t - 15) * 128]
                                    Rd = [R_Hc]
                                if k < 4:
                                    act(dst, pst[b][:, k, :], AF.Identity, [R_pst[b][0], R_modT], Rd,
                                        scale=A1s[:, k, mj:mj + 1], bias=modT[:, k, mj:mj + 1])
                                else:
                                    ts("dve", dst, pst[b][:, k, :], A1s[:, k, mj:mj + 1], modT[:, k, mj:mj + 1],
                                       ALU.mult, ALU.add, [R_pst[b][1], R_modT], Rd)

                        pipeline(18, [a1_L, a1_X])
                        S.barrier()
                    if stop_after == "A1":
                        S.barrier(); S.finish(); return nc

                    with ExitStack() as s2:
                        cosT = sb(s2, "cosT", [128, T], F32)
                        sinT = sb(s2, "sinT", [128, T], F32)
                        S.dma("sp", cosT[:], cosT_d, writes=[R_cs], nowaw=True)
                        S.dma("sp", sinT[:], sinT_d, writes=[R_cs], nowaw=True)
                        psq = [ps(s2, f"psq{i}", [128, 512]) for i in range(3)]
                        psr2 = [ps(s2, f"psr2{i}", [128, 512]) for i in range(3)]
                        R_psq = [Reg() for _ in range(3)]
                        R_psr2 = [Reg() for _ in range(3)]
                        qb = [sb(s2, f"qb{i}", [128, 512], BF16) for i in range(3)]
                        t1 = [sb(s2, f"t1{i}", [128, 512], F32) for i in range(3)]
                        t2 = [sb(s2, f"t2{i}", [128, 512], F32) for i in range(3)]
                        R_qb, R_t1, R_t2 = [Reg() for _ in range(3)], [Reg() for _ in range(3)], [Reg() for _ in range(3)]
                        u = [0]

                        pend = [None]

                        def rope_R(i, dst, R_dst, g):
                            mm(psr2[i][:], rmat[:], qb[i][:], True, True, [R_qb[i], R_c], [R_psr2[i]])
                            tt("dve", t1[i][:], psq[i][:], cosT[:, g * 512:(g + 1) * 512], ALU.mult, [R_psq[i], R_cs, R_qb[i]], [R_t1[i]])
                            tt("dve", t2[i][:], psr2[i][:], sinT[:, g * 512:(g + 1) * 512], ALU.mult, [R_psr2[i], R_cs], [R_t2[i]])
                            if isinstance(dst, tuple):
                                tt("dve", dst[0], t1[i][0:64, :], t2[i][0:64, :], ALU.add, [R_t1[i], R_t2[i]], R_dst, )
                                S.op("dve", lambda e: e.tensor_tensor(out=dst[1], in0=t1[i][64:128, :], in1=t2[i][64:128, :], op=ALU.add),
                                     [R_t1[i], R_t2[i]], R_dst, nowaw=True)
                            else:
                                tt("dve", dst, t1[i][:], t2[i][:], ALU.add, [R_t1[i], R_t2[i]], R_dst)

                        def flush_pend():
                            if pend[0] is not None:
                                rope_R(*pend[0])
                                pend[0] = None

                        def rope_unit(wt, Rw, col0, Hsrc, R_Hs, tok0, ntok, dst, R_dst, rope, g):
                            i = u[0] % 3
                            u[0] += 1
                            for k in range(8):
                                mm(psq[i][:, 0:ntok], wt[:, k, col0:col0 + 128], Hsrc[:, k, tok0:tok0 + ntok], k == 0, k == 7,
                                   [Rw] + R_Hs, [R_psq[i]])
                            if not rope:
                                if isinstance(dst, tuple):
                                    act(dst[0], psq[i][0:64, 0:ntok], AF.Copy, [R_psq[i]], R_dst)
                                    S.op("act", lambda e: e.activation(out=dst[1], in_=psq[i][64:128, 0:ntok], func=AF.Copy), [R_psq[i]], R_dst, nowaw=True)
                                else:
                                    act(dst, psq[i][:, 0:ntok], AF.Copy, [R_psq[i]], R_dst)
                                flush_pend()
                                return
                            act(qb[i][:], psq[i][:], AF.Copy, [R_psq[i]], [R_qb[i]])
                            flush_pend()
                            pend[0] = (i, dst, R_dst, g)

                        for blk in range(4):
                            wt, Rw = next_w([(0, winv[:, :, OFF_Q + blk * 256:OFF_Q + (blk + 1) * 256], 256)])
                            for cc in range(2):
                                c = blk * 2 + cc
                                for g in range(4):
                                    rope_unit(wt, Rw, cc * 128, H, [R_H[k][g] for k in range(8)], g * 512, 512,
                                              QO[:, c, g * 512:(g + 1) * 512], [R_Q[c][g * 4 + j] for j in range(4)], True, g)
                        for blk in range(2):
                            pieces = []
                            for hh in range(2):
                                kv = blk * 2 + hh
                                srcw = winv[:, :, OFF_K + kv * 64:OFF_K + (kv + 1) * 64]
                                pieces.append((hh * 128, srcw, 64))
                                pieces.append((hh * 128 + 64, srcw, 64))
                            wt, Rw = next_w(pieces)
                            for hh in range(2):
                                kv = blk * 2 + hh
                                for g in range(4):
                                    rope_unit(wt, Rw, hh * 128, H, [R_H[k][g] for k in range(8)], g * 512, 512,
                                              (KtA[0:64, kv, g * 512:(g + 1) * 512], KtB[64:128, kv, g * 512:(g + 1) * 512]), [R_K[kv][g * 4 + j] for j in range(4)], True, g)
                                rope_unit(wt, Rw, hh * 128, Hc, [R_Hc], 0, L, (KtA[0:64, kv, T:T + L], KtB[64:128, kv, T:T + L]), [R_K[kv][16], R_K[kv][17]], False, 0)
                        flush_pend()
                        wt, Rw = next_w([(0, winv[:, :, OFF_V:OFF_V + 256], 256)])
                        for t in range(18):
                            i = u[0] % 3
                            u[0] += 1
                            for k in range(8):
                                lhs = H[:, k, t * 128:(t + 1) * 128] if t < 16 else Hc[:, k, (t - 16) * 128:(t - 15) * 128]
                                Rl = [R_H[k][t // 4]] if t < 16 else [R_Hc]
                                mm(psq[i][:, 0:256], lhs, wt[:, k, 0:256], k == 0, k == 7, Rl + [Rw], [R_psq[i]])
                            cp("act" if t % 2 else "dve", V[:, t, :, 0:64], psq[i][:, 0:256].rearrange("p (g d) -> p g d", g=4),
                               [R_psq[i]], [R_V[t]])
                        S.barrier()
                    if dbg and s == 0:
                        dbg_dump("H", H[:], [r for rr in R_H for r in rr])
                        dbg_dump("Q", QO[:], [r for rr in R_Q for r in rr])
                        dbg_dump("Kt", KtA[:], [r for rr in R_K for r in rr])
                        dbg_dump("V", V[:], R_V)
                    if stop_after == "A2":
                        S.barrier()
                        S.finish()
                        return nc

                    with ExitStack() as s3:
                        pss = [ps(s3, f"pss{i}", [128, 512]) for i in range(4)]
                        R_pss = [Reg() for _ in range(4)]
                        pso = [ps(s3, f"pso{i}", [128, 4, 128]) for i in range(2)]
                        R_pso = [Reg() for _ in range(2)]
                        pstr = [ps(s3, f"pstr{i}", [128, 1024], BF16) for i in range(2)]
                        R_pstr = [Reg() for _ in range(2)]
                        PT = [[sb(s3, f"PT{hh}_{i}", [128, 384], BF16) for i in range(4)] for hh in range(2)]
                        R_PT = [[Reg() for _ in range(4)] for _ in range(2)]
                        PTc = [[[sb(s3, f"PTc{par}_{hh}_{cb}", [128, T], BF16) for cb in range(2)] for hh in range(2)] for par in range(2)]
                        R_PTc = [[[[Reg() for _ in range(4)] for _ in range(2)] for _ in range(2)] for _ in range(2)]
                        den = [sb(s3, f"den{i}", [128, 4], F32) for i in range(2)]
                        R_den = [Reg(), Reg()]
                        ob = [sb(s3, f"ob{i}", [128, 128], BF16) for i in range(2)]
                        R_ob = [Reg(), Reg()]
                        n_s = [0]

                        def ctx_unit(c, idx):
                            hh, rem = divmod(idx, 8)
                            cb, g = divmod(rem, 4)
                            kv = c // 2
                            p0 = hh * 64
                            i = n_s[0] % 4
                            n_s[0] += 1
                            mm(pss[i][:], Kt2[hh][:, kv, T + cb * 128:T + (cb + 1) * 128],
                               QO[:, c, g * 512:(g + 1) * 512], True, True,
                               [R_K[kv][16 + cb]] + [R_Q[c][g * 4 + j] for j in range(4)], [R_pss[i]])
                            act(PTc[c % 2][hh][cb][:, g * 512:(g + 1) * 512], pss[i][:], AF.Exp, [R_pss[i]],
                                [R_PTc[c % 2][hh][cb][g]], scale=0.125)

                        def score_unit(c, j):
                            kv = c // 2
                            qlo, qhi = max(j - 1, 0), min(j + 1, 15)
                            n = (qhi - qlo + 1) * 128
                            for hh in range(2):
                                p0 = hh * 64
                                i = n_s[0] % 4
                                n_s[0] += 1
                                nmask = (1 if j >= 1 else 0) + (1 if j <= 14 else 0)
                                mm(pss[i][:, 0:n], Kt2[hh][:, kv, j * 128:(j + 1) * 128], QO[:, c, qlo * 128:(qhi + 1) * 128],
                                   True, nmask == 0, [R_K[kv][j]] + [R_Q[c][q] for q in range(qlo, qhi + 1)], [R_pss[i]])
                                km = 0
                                if j >= 1:
                                    km += 1
                                    mm(pss[i][:, 0:128], identb[:], mle[:], False, km == nmask, [R_c], [R_pss[i]])
                                if j <= 14:
                                    km += 1
                                    mm(pss[i][:, n - 128:n], identb[:], mge[:], False, km == nmask, [R_c], [R_pss[i]])
                                act(PT[hh][j % 4][:, 0:n], pss[i][:, 0:n], AF.Exp, [R_pss[i]], [R_PT[hh][j % 4]], scale=0.125)

                        def pv_mm(c, iq):
                            kv = c // 2
                            io = iq % 2
                            for hh in range(2):
                                jl = [j for j in (iq - 1, iq, iq + 1) if 0 <= j < 16]
                                nmm = len(jl) + 2
                                cnt = 0
                                for j in jl:
                                    qlo = max(j - 1, 0)
                                    off = (iq - qlo) * 128
                                    cnt += 1
                                    mm(pso[io][:, hh, 0:65], PT[hh][j % 4][:, off:off + 128], V[:, j, kv, :], cnt == 1, cnt == nmm,
                                       [R_PT[hh][j % 4], R_V[j]], [R_pso[io]])
                                for cb in range(2):
                                    cnt += 1
                                    mm(pso[io][:, hh, 0:65], PTc[c % 2][hh][cb][:, iq * 128:(iq + 1) * 128], V[:, 16 + cb, kv, :], cnt == 1, cnt == nmm,
                                       [R_PTc[c % 2][hh][cb][iq // 4], R_V[16 + cb]], [R_pso[io]])
                            d = den[io]
                            tt("dve", d[:, 0:2], pso[io][:, 0:2, 64], esink[:, 2 * c:2 * c + 2], ALU.add, [R_pso[io], R_c], [R_den[io]])
                            S.op("dve", lambda e: e.reciprocal(out=d[:, 2:4], in_=d[:, 0:2]), [R_den[io]], [R_den[io]])
                            for hh in range(2):
                                ts("dve", ob[io][:, hh * 64:(hh + 1) * 64], pso[io][:, hh, 0:64], d[:, 2 + hh:3 + hh], None, ALU.mult, None,
                                   [R_pso[io], R_den[io]], [R_ob[io]])

                        def pv_fin(c, iq):
                            io = iq % 2
                            tr(pstr[io][:, 0:128], ob[io][:], identb[:], [R_ob[io], R_c], [R_pstr[io]])
                            cp("act" if iq % 2 else "dve", QO[:, c, iq * 128:(iq + 1) * 128], pstr[io][:, 0:128], [R_pstr[io]], [R_Q[c][iq]])

                        for idx in range(16):
                            ctx_unit(0, idx)
                        NU = 8 * 16
                        for u in range(NU + 3):
                            if u < NU:
                                c, j = divmod(u, 16)
                                score_unit(c, j)
                                if c + 1 < 8:
                                    ctx_unit(c + 1, j)
                            if 0 <= u - 2 < NU:
                                pv_mm(*divmod(u - 2, 16))
                            if 0 <= u - 3 < NU:
                                pv_fin(*divmod(u - 3, 16))
                        S.barrier()
                if dbg and s == 0:
                    dbg_dump("O", QO[:], [r for rr in R_Q for r in rr])
                if stop_after == "A3":
                    S.barrier()
                    S.finish()
                    return nc

                Y = sb(sa, "Y", [128, 8, T], BF16)
                R_Y = [Reg() for _ in range(8)]
                with ExitStack() as s4:
                    psu = [ps(s4, f"psu{i}", [128, 512]) for i in range(6)]
                    R_psu = [Reg() for _ in range(6)]
                    zb = [sb(s4, f"zb{i}", [128, T + 2], F32) for i in range(2)]
                    R_zb = [Reg(), Reg()]
                    Bs = [sb(s4, f"Bs{i}", [128, T], F32) for i in range(2)]
                    R_Bs = [Reg(), Reg()]
                    us = [sb(s4, f"us{i}", [128, 512], F32) for i in range(2)]
                    R_us = [Reg(), Reg()]
                    acc = sb(s4, "acc", [128, T], F32)
                    R_acc = Reg()
                    for i in range(2):
                        S.op("pool", lambda e, i=i: e.memset(zb[i][:, 0:1], 0.0), writes=[R_zb[i]])
                        S.op("pool", lambda e, i=i: e.memset(zb[i][:, T + 1:T + 2], 0.0), writes=[R_zb[i]])
                    n_u = 0
                    ring4 = (wring + [sb(s4, f"wr4_{i}", [128, 8, 256], BF16) for i in range(2)], R_wr + [Reg() for _ in range(2)], [0])

                    def load4(blk):
                        return [next_w([(0, winv[:, :, off + blk * 256:off + (blk + 1) * 256], 256)], ring=ring4) for off in (OFF_U, OFF_C, OFF_B)]

                    nxt4 = load4(0)
                    for blk in range(4):
                        wts = nxt4
                        if blk + 1 < 4:
                            nxt4 = load4(blk + 1)
                        for cc in range(2):
                            c = blk * 2 + cc
                            z = zb[c % 2]
                            Rz = R_zb[c % 2]
                            Bt = Bs[c % 2]
                            RB = R_Bs[c % 2]
                            for g in range(4):
                                pp = []
                                for (wt, Rw) in wts:
                                    i = n_u % 6
                                    n_u += 1
                                    for k in range(8):
                                        mm(psu[i][:], wt[:, k, cc * 128:(cc + 1) * 128], H[:, k, g * 512:(g + 1) * 512], k == 0, k == 7,
                                           [Rw, R_H[k][g]], [R_psu[i]])
                                    pp.append(i)
                                ui = (c * 4 + g) % 2
                                cp("act", us[ui][:], psu[pp[0]][:], [R_psu[pp[0]]], [R_us[ui]])
                                tt("dve", z[:, 1 + g * 512:1 + (g + 1) * 512], psu[pp[1]][:], us[ui][:], ALU.mult,
                                   [R_psu[pp[1]], R_us[ui]], [Rz])
                                cp("act", Bt[:, g * 512:(g + 1) * 512], psu[pp[2]][:], [R_psu[pp[2]]], [RB])
                            ts("dve", acc[:], z[:, 1:T + 1], convw[:, c, 1:2], None, ALU.mult, None, [Rz, R_c], [R_acc])
                            stt(acc[:], z[:, 0:T], convw[:, c, 0:1], acc[:], ALU.mult, ALU.add, [Rz, R_c, R_acc], [R_acc])
                            stt(acc[:], z[:, 2:T + 2], convw[:, c, 2:3], acc[:], ALU.mult, ALU.add, [Rz, R_c, R_acc], [R_acc])
                            tt("dve", Y[:, c, :], acc[:], Bt[:], ALU.mult, [R_acc, RB], [R_Y[c]])
                    S.barrier()
                if dbg and s == 0:
                    dbg_dump("Y", Y[:], R_Y)
                if stop_after == "A4":
                    S.barrier()
                    S.finish()
                    return nc

                R_md = [[Reg() for _ in range(4)] for _ in range(8)]
                with ExitStack() as s5:
                    psm = [ps(s5, f"psm{i}", [128, 512]) for i in range(8)]
                    R_psm = [Reg() for _ in range(8)]
                    sg = [sb(s5, f"sg{i}", [128, 512], F32) for i in range(4)]
                    R_sg = [Reg() for _ in range(4)]
                    tm = [sb(s5, f"tm{i}", [128, 512], F32) for i in range(4)]
                    R_tm = [Reg() for _ in range(4)]
                    mst = [sb(s5, f"mst{i}", [128, 512], BF16) for i in range(2)]
                    R_mst = [Reg(), Reg()]
                    wpav = wpa_d.rearrange("(k p) n -> p k n", p=128)
                    wpcv = wpc_d.rearrange("(k p) n -> p k n", p=128)
                    n_m = 0
                    n_g = 0
                    for blk in range(4):
                        if blk == 0:
                            ring5 = (wring + [sb(s5, f"wr5_{i}", [128, 8, 256], BF16) for i in range(4)], R_wr + [Reg() for _ in range(4)], [0])

                            def load5(b_):
                                return [
                                    (next_w([(0, winv[:, :, OFF_GA + b_ * 256:OFF_GA + (b_ + 1) * 256], 256)], ring=ring5), H, R_H, None),
                                    (next_w([(0, wpav[:, :, b_ * 256:(b_ + 1) * 256], 256)], ring=ring5), QO, None, R_Q),
                                    (next_w([(0, winv[:, :, OFF_GC + b_ * 256:OFF_GC + (b_ + 1) * 256], 256)], ring=ring5), H, R_H, None),
                                    (next_w([(0, wpcv[:, :, b_ * 256:(b_ + 1) * 256], 256)], ring=ring5), Y, None, None),
                                ]
                            nxt5 = load5(0)
                        wts = nxt5
                        if blk + 1 < 4:
                            nxt5 = load5(blk + 1)
                        for cc in range(2):
                            m = blk * 2 + cc
                            for g in range(4):
                                pp = []
                                for wi_, ((wt, Rw), src, RH_, RQ_) in enumerate(wts):
                                    i = n_m % 8
                                    n_m += 1
                                    for k in range(8):
                                        if RH_ is not None:
                                            rr = [RH_[k][g]]
                                        elif RQ_ is not None:
                                            rr = [RQ_[k][g * 4 + j] for j in range(4)]
                                        else:
                                            rr = [R_Y[k]]
                                        mm(psm[i][:], wt[:, k, cc * 128:(cc + 1) * 128], src[:, k, g * 512:(g + 1) * 512], k == 0, k == 7,
                                           [Rw] + rr, [R_psm[i]])
                                    pp.append(i)
                                a0, a1 = n_g % 4, (n_g + 1) % 4
                                n_g += 2
                                act(sg[a0][:], psm[pp[0]][:], AF.Sigmoid, [R_psm[pp[0]]], [R_sg[a0]])
                                act(sg[a1][:], psm[pp[2]][:], AF.Sigmoid, [R_psm[pp[2]]], [R_sg[a1]])
                                tt("dve", tm[a0][:], psm[pp[1]][:], sg[a0][:], ALU.mult, [R_psm[pp[1]], R_sg[a0]], [R_tm[a0]])
                                tt("dve", tm[a1][:], psm[pp[3]][:], sg[a1][:], ALU.mult, [R_psm[pp[3]], R_sg[a1]], [R_tm[a1]])
                                mi = (m * 4 + g) % 2
                                tt("dve", mst[mi][:], tm[a0][:], tm[a1][:], ALU.add, [R_tm[a0], R_tm[a1]], [R_mst[mi]])
                                S.dma(STQ, md_d[s, m, :, g * 512:(g + 1) * 512], mst[mi][:], reads=[R_mst[mi]], writes=[R_md[m][g]])
                    S.barrier()
            S.barrier()
            if stop_after == "A5":
                S.finish()
                return nc

            with ExitStack() as s6:
                wo = sb(s6, "wo", [128, 8, D], BF16)
                R_wo = Reg()
                stg6 = [sb(s6, f"stg6{i}", [128, 8, 256], F32) for i in range(2)]
                R_stg6 = [Reg(), Reg()]
                woutv = wout_d.rearrange("(k p) n -> p k n", p=128)
                for blk in range(4):
                    load_w(stg6, R_stg6, wo[:, :, blk * 256:(blk + 1) * 256], [(0, woutv[:, :, blk * 256:(blk + 1) * 256], 256)], R_wo, 256)
                g1bc = sb(s6, "g1bc", [128, D], F32)
                A2bc = sb(s6, "A2bc", [128, D], F32)
                sh2bc = sb(s6, "sh2bc", [128, D], F32)
                R_bc = Reg()
                S.dma("sp", g1bc[:], modrows_d[s:s + 1, 2 * D:3 * D].to_broadcast([128, D]), reads=[R_modrows], writes=[R_bc], nowaw=True)
                S.dma("sp", sh2bc[:], modrows_d[s:s + 1, 3 * D:4 * D].to_broadcast([128, D]), reads=[R_modrows], writes=[R_bc], nowaw=True)
                S.dma("sp", A2bc[:], modrows_d[s:s + 1, 4 * D:5 * D].to_broadcast([128, D]), reads=[R_modrows], writes=[R_bc], nowaw=True)
                n2bc = sb(s6, "n2bc", [128, D], F32)
                S.dma("sp", n2bc[:], n2g_d.to_broadcast([128, D]), writes=[R_bc], nowaw=True)
                stt(A2bc[:], A2bc[:], 1.0, n2bc[:], ALU.add, ALU.mult, [R_bc], [R_bc])
                Mg = [sb(s6, f"Mg{i}", [128, 8, 512], BF16) for i in range(2)]
                R_Mg = [Reg(), Reg()]
                pso6 = [ps(s6, f"pso6{i}", [128, D]) for i in range(2)]
                R_pso6 = [Reg(), Reg()]
                pst6 = ps(s6, "pst6", [128, 8, 128])
                R_pst6 = Reg()
                psl = ps(s6, "psl", [128, 512])
                R_psl = Reg()
                xr = [sb(s6, f"xr{i}", [128, D], F32) for i in range(2)]
                R_xr = [Reg(), Reg()]
                x1t = [sb(s6, f"x1t{i}", [128, D], F32) for i in range(2)]
                R_x1t = [Reg(), Reg()]
                tmp6_ = [sb(s6, f"tmp6{i}", [128, D], F32) for i in range(2)]
                R_tmp6_ = [Reg(), Reg()]
                hxf_ = [sb(s6, f"hxf{i}", [128, D], F32) for i in range(2)]
                R_hxf_ = [Reg(), Reg()]
                hxb = [sb(s6, f"hxb{i}", [128, D], BF16) for i in range(2)]
                R_hxb = [Reg(), Reg()]
                hxT_ = [sb(s6, f"hxT{i}", [128, 8, 128], F32) for i in range(2)]
                R_hxT_ = [Reg(), Reg()]
                junk6_ = [sb(s6, f"junk6{i}", [128, D], F32) for i in range(2)]
                R_junk6_ = [Reg(), Reg()]
                stat6 = [sb(s6, f"stat6{i}", [128, 2], F32) for i in range(2)]
                R_st6 = [Reg(), Reg()]
                lg = sb(s6, "lg", [64, 512], F32)
                R_lg = Reg()
                r0 = 0 if s == 0 else 32
                tmpB_ = [sb(s6, f"tmpB{i}", [128, D], F32) for i in range(2)]
                R_tmpB_ = [Reg(), Reg()]
                psl2 = ps(s6, "psl2", [128, 512])
                psl_ = [psl, psl2]
                R_psl_ = [R_psl, Reg()]

                def a6_A(t):
                    g, tt_ = divmod(t, 4)
                    b = t % 2
                    Mt, RM = Mg[g % 2], R_Mg[g % 2]
                    if tt_ == 0:
                        S.dma("sp", Mt[:], md_d[s, :, :, g * 512:(g + 1) * 512].rearrange("m p t -> p m t"),
                              reads=[R_md[m][g] for m in range(8)], writes=[RM])
                    tmp6, R_tmp6, junk6, R_junk6 = tmp6_[b], R_tmp6_[b], junk6_[b], R_junk6_[b]
                    S.dma("sp", xr[b][:], x_d[s, t * 128:(t + 1) * 128, :], writes=[R_xr[b]])
                    for nb in range(2):
                        for k in range(8):
                            mm(pso6[b][:, nb * 512:(nb + 1) * 512], Mt[:, k, tt_ * 128:(tt_ + 1) * 128], wo[:, k, nb * 512:(nb + 1) * 512],
                               k == 0, k == 7, [RM, R_wo], [R_pso6[b]])
                    tt("dve", tmp6[:], pso6[b][:], g1bc[:], ALU.mult, [R_pso6[b], R_bc], [R_tmp6])
                    tt("dve", x1t[b][:], tmp6[:], xr[b][:], ALU.add, [R_tmp6, R_xr[b]], [R_x1t[b]])
                    S.dma(STQ, x1_d[s, t * 128:(t + 1) * 128, :], x1t[b][:], reads=[R_x1t[b]], writes=[R_x1d], nowaw=True)
                    rms_rstd("a6", x1t[b][:], R_x1t[b], junk6[:], R_junk6, stat6[b][:, 0:1], stat6[b][:, 1:2], R_st6[b], recip=False)

                def a6_B(t):
                    g, tt_ = divmod(t, 4)
                    b = t % 2
                    tmpB, R_tmpB, hxf, R_hxf, hxT, R_hxT = tmpB_[b], R_tmpB_[b], hxf_[b], R_hxf_[b], hxT_[b], R_hxT_[b]
                    pl, Rpl = psl_[g % 2], R_psl_[g % 2]
                    S.op("dve", lambda e: e.reciprocal(out=stat6[b][:, 1:2], in_=stat6[b][:, 0:1]), [R_st6[b]], [R_st6[b]])
                    stt(tmpB[:], x1t[b][:], stat6[b][:, 1:2], A2bc[:], ALU.mult, ALU.mult, [R_x1t[b], R_st6[b], R_bc], [R_tmpB])
                    tt("dve", hxf[:], tmpB[:], sh2bc[:], ALU.add, [R_tmpB, R_bc], [R_hxf])
                    cp("act", hxb[b][:], hxf[:], [R_hxf], [R_hxb[b]])
                    S.dma(STQ, hx2_d[s, t * 128:(t + 1) * 128, :], hxb[b][:], reads=[R_hxb[b]], writes=[R_hx2d], nowaw=True)

                def a6_T(t):
                    b = t % 2
                    hxf, R_hxf, hxT, R_hxT = hxf_[b], R_hxf_[b], hxT_[b], R_hxT_[b]
                    for k in range(8):
                        tr(pst6[:, k, :], hxf[:, k * 128:(k + 1) * 128], identf[:], [R_hxf, R_c], [R_pst6])
                    cp("dve", hxT[:], pst6[:], [R_pst6], [R_hxT])

                def a6_C(t):
                    g, tt_ = divmod(t, 4)
                    b = t % 2
                    hxT, R_hxT = hxT_[b], R_hxT_[b]
                    pl, Rpl = psl_[g % 2], R_psl_[g % 2]
                    for k in range(8):
                        mm(pl[:, tt_ * 128:(tt_ + 1) * 128], wr_f[:, k, :], hxT[:, k, :], k == 0, k == 7, [R_c, R_hxT], [Rpl])
                    if tt_ == 3:
                        act(lg[r0:r0 + 16, :], pl[r0:r0 + 16, :], AF.Exp, [Rpl], [R_lg])
                        mm(pl[r0:r0 + 16, :], ones16[r0:r0 + 16, r0:r0 + 16], lg[r0:r0 + 16, :], True, True, [R_lg, R_c], [Rpl])
                        S.op("dve", lambda e, g=g: e.reciprocal(out=aff[r0:r0 + 16, g * 512:(g + 1) * 512], in_=pl[r0:r0 + 16, :]), [Rpl], [R_aff])
                        tt("dve", aff[r0:r0 + 16, g * 512:(g + 1) * 512], aff[r0:r0 + 16, g * 512:(g + 1) * 512], lg[r0:r0 + 16, :], ALU.mult,
                           [R_aff, R_lg], [R_aff])

                pipeline(16, [a6_A, a6_B, a6_T, a6_C])
                S.barrier()
            S.barrier()
            if stop_after == "A6" and s == 0:
                dbg_dump("aff", aff[:], [R_aff])
                S.barrier()
                S.finish()
                return nc

        postok = sb(top, "postok", [128, 16, 64], F32)
        gatehl = sb(top, "gatehl", [128, 16, 64, 2], BF16)
        R_pt = Reg()
        with ExitStack() as sr:
            work = sb(sr, "work", [64, T], F32)
            mx = sb(sr, "mx", [64, 8], F32)
            mask = sb(sr, "mask", [64, T], F32)
            posm = sb(sr, "posm", [64, T], F32)
            gate = sb(sr, "gate", [64, T], F32)
            posb = sb(sr, "posb", [64, T], BF16)
            gtok = sb(sr, "gtok", [128, 16, 64], F32)
            glo = sb(sr, "glo", [128, 16, 64], F32)
            psT = [ps(sr, f"psT{i}", [128, 8, 64]) for i in range(4)]
            R_w, R_mx, R_mask, R_posm, R_gate, R_psT = Reg(), Reg(), Reg(), Reg(), Reg(), [Reg() for _ in range(4)]
            cp("dve", work[:], aff[:], [R_aff], [R_w])
            for r in range(1 if skip_pre else CAP // 8):
                S.op("dve", lambda e: e.max(out=mx[:], in_=work[:]), [R_w], [R_mx])
                if r < CAP // 8 - 1:
                    S.op("dve", lambda e: e.match_replace(out=work[:], in_to_replace=mx[:], in_values=work[:], imm_value=-1.0), [R_w, R_mx], [R_w])
            ts("dve", mask[:], aff[:], mx[:, 7:8], None, ALU.is_ge, None, [R_aff, R_mx], [R_mask])
            S.op("dve", lambda e: e.memset(work[:], 1.0), [R_mx], [R_w])
            S.op("dve", lambda e: e.tensor_tensor_scan(out=posm[:], data0=work[:], data1=mask[:], initial=0.0, op0=ALU.mult, op1=ALU.add),
                 [R_mask, R_w], [R_posm])
            tt("dve", posm[:], posm[:], mask[:], ALU.mult, [R_posm, R_mask], [R_posm])
            ts("dve", posm[:], posm[:], -1.0, None, ALU.add, None, [R_posm], [R_posm])
            tt("dve", gate[:], aff[:], mask[:], ALU.mult, [R_aff, R_mask], [R_gate])
            cp("dve", posb[:], posm[:], [R_posm], [R_posm])
            R_posr = Reg()
            for s in range(NS):
                S.dma("sp", posr_d[s], posb[s * 32:s * 32 + 16, :], reads=[R_posm], writes=[R_posr], nowaw=True)
            for half in range(2):
                for which, src, dst in ((0, posm, postok), (1, gate, gtok)):
                    p = psT[half * 2 + which]
                    Rp = R_psT[half * 2 + which]
                    for tl in range(8):
                        t = half * 8 + tl
                        tr(p[:, tl, :], src[:, t * 128:(t + 1) * 128], identf[0:64, 0:64], [R_posm, R_gate, R_c], [Rp])
                    cp("act" if which else "dve", dst[:, half * 8:(half + 1) * 8, :], p[:], [Rp], [R_pt])
            cp("dve", gatehl[:, :, :, 0], gtok[:], [R_pt], [R_pt])
            tt("dve", glo[:], gtok[:], gatehl[:, :, :, 0], ALU.subtract, [R_pt], [R_pt])
            cp("dve", gatehl[:, :, :, 1], glo[:], [R_pt], [R_pt])
            if dbg:
                dbg_dump("posm", posm[:], [R_posm])
                dbg_dump("gate", gate[:], [R_gate])
            S.barrier()
        S.barrier()
        if stop_after == "R":
            S.finish()
            return nc

        with ExitStack() as sm:
            NW = 6
            wslot = [sb(sm, f"wslot{i}", [128, 8, D], BF16) for i in range(NW)]
            R_ws = [Reg() for _ in range(NW)]
            stgm = [sb(sm, f"stgm{i}", [128, 2, D], F32) for i in range(2)]
            R_stgm = [Reg() for _ in range(2)]
            mcast = [0]
            tokhl = sb(sm, "tokhl", [128, 16, 2], BF16)
            R_tok = Reg()
            S.dma("sp", tokhl[:], tokhl_d, writes=[R_tok])
            gt4 = sb(sm, "gt4", [128, 16, 64, 4], BF16)
            R_gt4 = Reg()
            cp("dve", gt4[:, :, :, 0:2], gatehl[:], [R_pt], [R_gt4])
            for r_ in range(64):
                S.op("dve", lambda en, r_=r_: en.tensor_copy(out=gt4[:, :, r_, 2:4], in_=tokhl[:]), [R_tok], [R_gt4], nowaw=True)
            St = [sb(sm, f"St{i}", [128, 16, CAP], BF16) for i in range(2)]
            R_St = [Reg(), Reg()]
            xtok = [sb(sm, f"xtok{i}", [128, 2, D], BF16) for i in range(3)]
            R_xtok = [Reg() for _ in range(3)]
            xeT_ = [sb(sm, f"xeT{i}", [128, 8, CAP], BF16) for i in range(2)]
            R_xe_ = [Reg(), Reg()]
            hT = sb(sm, "hT", [128, 8, CAP], BF16)
            R_hT = Reg()
            sa_ = [sb(sm, f"sa{i}", [128, CAP], F32) for i in range(2)]
            R_sa = [Reg(), Reg()]
            gsl_ = [sb(sm, f"gsl{i}", [128, 2, 4], F32) for i in range(3)]
            gs1_ = [sb(sm, f"gs1{i}", [128, 2], F32) for i in range(3)]
            idxf_ = [sb(sm, f"idxf{i}", [128, 2], F32) for i in range(3)]
            idxu_ = [sb(sm, f"idxu{i}", [128, 2], mybir.dt.uint32) for i in range(3)]
            R_gs_ = [Reg() for _ in range(3)]
            yes = [sb(sm, f"yes{i}", [128, D], F32) for i in range(2)]
            R_yes = [Reg(), Reg()]
            g2bc = sb(sm, "g2bcM", [128, NS, D], F32)
            R_g2 = Reg()
            for s_ in range(NS):
                S.dma("sp", g2bc[:, s_, :], modrows_d[s_:s_ + 1, 5 * D:6 * D].to_broadcast([128, D]), reads=[R_modrows], writes=[R_g2], nowaw=True)
            x1_flat = x1_d.rearrange("s t d -> (s t) d")
            R_acc = [Reg() for _ in range(NS)]
            psx = [ps(sm, f"psx{i}", [128, 1024], BF16) for i in range(2)]
            R_psx = [Reg(), Reg()]
            psa = [ps(sm, f"psa{i}", [128, 512]) for i in range(2)]
            R_psa = [Reg(), Reg()]
            psg = ps(sm, "psg", [128, 2, 256])
            R_psg = Reg()
            psy = [ps(sm, f"psy{i}", [128, D]) for i in range(1)]
            R_psy = [Reg()]
            R_ye = Reg()
            wsel = [0]
            wq = []

            pend_cast = [None]

            def load_expert_mat(src_d):
                i = wsel[0] % NW
                wsel[0] += 1
                v = src_d.rearrange("(k p) n -> p k n", p=128)
                tok = {"left": 4}
                for blk in range(4):
                    j = mcast[0]
                    mcast[0] += 1
                    stg_, Rs_ = stgm[j % 2], R_stgm[j % 2]

                    def emit_dma(blk=blk, stg_=stg_, Rs_=Rs_):
                        S.dma("sp", stg_[:], v[:, 2 * blk:2 * blk + 2, :], writes=[Rs_])

                    def emit_cast(blk=blk, stg_=stg_, Rs_=Rs_, j=j):
                        cp(("act", "dve")[(j // 2) % 2], wslot[i][:, 2 * blk:2 * blk + 2, :], stg_[:], [Rs_], [R_ws[i]], nowaw=True)
                        tok["left"] -= 1

                    wq.append(emit_dma)
                    if pend_cast[0] is not None:
                        wq.append(pend_cast[0])
                    pend_cast[0] = emit_cast
                return wslot[i], R_ws[i], tok

            def drain(n=1):
                for _ in range(n):
                    if wq:
                        wq.pop(0)()

            def ensure(tok):
                while tok["left"] > 0:
                    if wq:
                        wq.pop(0)()
                    else:
                        pend_cast[0]()
                        pend_cast[0] = None

            units = [(e, s) for e in range(NE) for s in range(NS)]
            NU = len(units)
            hx2_flat = hx2_d.rearrange("s t d -> (s t) d")

            def m_P0(u):
                e, s = units[u]
                row = s * 32 + e
                Sx, RS = St[u % 2], R_St[u % 2]
                ops = []
                for t in range(16):
                    ops.append(lambda t=t: S.op("dve", lambda en: en.tensor_scalar(out=Sx[:, t, :], in0=iota[:], scalar1=postok[:, t, row:row + 1],
                                                                                  scalar2=None, op0=ALU.is_equal), [R_pt, R_c], [RS], nowaw=True))
                return ops

            def m_P1(u):
                e, s = units[u]
                row = s * 32 + e
                Sx, RS = St[u % 2], R_St[u % 2]
                k3 = u % 3
                for half in range(2):
                    for t in range(16):
                        mm(psg[:, half, 0:4], Sx[:, t, half * 128:(half + 1) * 128], gt4[:, t, row, :], t == 0, t == 15, [RS, R_gt4], [R_psg])
                cp("dve", gsl_[k3][:], psg[:, :, 0:4], [R_psg], [R_gs_[k3]])
                tt("dve", gs1_[k3][:], gsl_[k3][:, :, 0], gsl_[k3][:, :, 1], ALU.add, [R_gs_[k3]], [R_gs_[k3]])
                stt(idxf_[k3][:], gsl_[k3][:, :, 2], float(s * T), gsl_[k3][:, :, 3], ALU.add, ALU.add, [R_gs_[k3]], [R_gs_[k3]])
                cp("dve", idxu_[k3][:], idxf_[k3][:], [R_gs_[k3]], [R_gs_[k3]])
                for half in range(2):
                    S.dma_custom("pool", lambda en, half=half: en.indirect_dma_start(
                        out=xtok[k3][:, half, :], out_offset=None, in_=hx2_flat,
                        in_offset=bass.IndirectOffsetOnAxis(ap=idxu_[k3][:, half:half + 1], axis=0)),
                        reads=[R_gs_[k3], R_hx2d], writes=[R_xtok[k3]], nowaw=(half == 1))

            def m_P2(u):
                k3 = u % 3
                xe, Rxe = xeT_[u % 2], R_xe_[u % 2]
                for half in range(2):
                    for dk in range(8):
                        tr(psx[half][:, dk * 128:(dk + 1) * 128], xtok[k3][:, half, dk * 128:(dk + 1) * 128], identb[:],
                           [R_xtok[k3], R_c], [R_psx[half]])
                    S.op("act", lambda en, half=half: en.activation(out=xe[:, :, half * 128:(half + 1) * 128],
                                                                   in_=psx[half][:].rearrange("p (k j) -> p k j", k=8), func=AF.Copy),
                         [R_psx[half]], [Rxe], nowaw=(half == 1))

            n_a = [0]
            n_y = [0]
            Wcur = {}

            def m_Fgu(u, fillers):
                e, s = units[u]
                xeT, R_xe = xeT_[u % 2], R_xe_[u % 2]
                if s == 0:
                    Wcur["g"], Wcur["u"], Wcur["d"] = Wn["g"], Wn["u"], Wn["d"]
                    if e + 1 < NE:
                        Wn["g"] = load_expert_mat(weg_d[e + 1])
                        Wn["u"] = load_expert_mat(weu_d[e + 1])
                        Wn["d"] = load_expert_mat(wed_d[e + 1])
                Wg, RWg, tokg = Wcur["g"]
                Wu, RWu, toku = Wcur["u"]
                Wd, RWd, tokd = Wcur["d"]
                ensure(tokg)
                ensure(toku)
                for mc in range(8):
                    i = n_a[0] % 2
                    n_a[0] += 1
                    for k in range(8):
                        mm(psa[i][:, 0:256], Wg[:, k, mc * 128:(mc + 1) * 128], xeT[:, k, :], k == 0, k == 7, [RWg, R_xe], [R_psa[i]])
                    for k in range(8):
                        mm(psa[i][:, 256:512], Wu[:, k, mc * 128:(mc + 1) * 128], xeT[:, k, :], k == 0, k == 7, [RWu, R_xe], [R_psa[i]])
                    act(sa_[i][:], psa[i][:, 0:256], AF.Silu, [R_psa[i]], [R_sa[i]])
                    tt("dve", hT[:, mc, :], psa[i][:, 256:512], sa_[i][:], ALU.mult, [R_psa[i], R_sa[i]], [R_hT])
                    for _ in range(2):
                        if fillers:
                            fillers.pop(0)()
                    if mc % 2 == 1:
                        drain(2)

            def m_Fdn(u):
                e, s = units[u]
                k3 = u % 3
                gs1, R_gs = gs1_[k3], R_gs_[k3]
                Wd, RWd, tokd = Wcur["d"]
                ensure(tokd)
                for half in range(2):
                    yb = yes[n_y[0] % 2]
                    Ry = R_yes[n_y[0] % 2]
                    n_y[0] += 1
                    for nb in range(2):
                        if half == 0:
                            dst, Rd = psy[0][:, nb * 512:(nb + 1) * 512], R_psy[0]
                        else:
                            dst, Rd = psa[nb][:], R_psa[nb]
                        for k in range(8):
                            mm(dst, hT[:, k, half * 128:(half + 1) * 128], Wd[:, k, nb * 512:(nb + 1) * 512], k == 0, k == 7, [R_hT, RWd], [Rd])
                    if half == 0:
                        act(yb[:], psy[0][:], AF.Copy, [R_psy[0], R_gs], [Ry], scale=gs1[:, half:half + 1])
                    else:
                        for nb in range(2):
                            S.op("act", lambda en, nb=nb: en.activation(out=yb[:, nb * 512:(nb + 1) * 512], in_=psa[nb][:], func=AF.Copy, scale=gs1[:, half:half + 1]),
                                 [R_psa[nb], R_gs], [Ry], nowaw=(nb == 1))
                    tt("dve", yb[:], yb[:], g2bc[:, s, :], ALU.mult, [Ry, R_g2], [Ry])
                    S.dma_custom("pool", lambda en, half=half, yb=yb: en.indirect_dma_start(
                        out=x1_flat, out_offset=bass.IndirectOffsetOnAxis(ap=idxu_[k3][:, half:half + 1], axis=0),
                        in_=yb[:], in_offset=None, compute_op=ALU.add),
                        reads=[Ry, R_gs, R_x1d], writes=[R_acc[s]], nowaw=(half == 1))
                    drain(2)

            Wn = {"g": load_expert_mat(weg_d[0]), "u": load_expert_mat(weu_d[0]), "d": load_expert_mat(wed_d[0])}
            drain(len(wq))
            for step in range(NU + 3):
                fillers = m_P0(step) if step < NU else []
                if 0 <= step - 3 < NU:
                    m_Fgu(step - 3, fillers)
                while fillers:
                    fillers.pop(0)()
                if 0 <= step - 1 < NU:
                    m_P1(step - 1)
                if 0 <= step - 2 < NU:
                    m_P2(step - 2)
                if 0 <= step - 3 < NU:
                    m_Fdn(step - 3)
            drain(len(wq))
            S.barrier()
        S.barrier()

        if stop_after == "M":
            S.finish()
            return nc

        with ExitStack() as sc:
            fgbc = sb(sc, "fgbc", [128, D], F32)
            R_bc2 = Reg()
            S.dma("sp", fgbc[:], fng_d.to_broadcast([128, D]), writes=[R_bc2])
            x1r = [sb(sc, f"x1r{i}", [128, D], F32) for i in range(3)]
            R_x1r = [Reg(), Reg(), Reg()]
            junkc = [sb(sc, f"junkc{i}", [128, D], F32) for i in range(2)]
            R_junkc = [Reg(), Reg()]
            statc = [sb(sc, f"statc{i}", [128, 2], F32) for i in range(3)]
            R_stc = [Reg(), Reg(), Reg()]
            ot = [sb(sc, f"ot{i}", [128, D], F32) for i in range(3)]
            R_ot = [Reg(), Reg(), Reg()]
            R_out = Reg()
            tiles = [(s, t) for s in range(NS) for t in range(16)]

            def c_L(i):
                s, t = tiles[i]
                k = i % 3
                S.dma("sp", x1r[k][:], x1_d[s, t * 128:(t + 1) * 128, :], reads=[R_x1d, R_acc[s]], writes=[R_x1r[k]])
                rms_rstd("c", x1r[k][:], R_x1r[k], junkc[i % 2][:], R_junkc[i % 2], statc[k][:, 0:1], statc[k][:, 1:2], R_stc[k], recip=False)

            def c_T(i):
                s, t = tiles[i]
                k = i % 3
                S.op("dve", lambda e: e.reciprocal(out=statc[k][:, 1:2], in_=statc[k][:, 0:1]), [R_stc[k]], [R_stc[k]])
                stt(ot[k][:], x1r[k][:], statc[k][:, 1:2], fgbc[:], ALU.mult, ALU.mult, [R_x1r[k], R_stc[k], R_bc2], [R_ot[k]])
                S.dma(STQ, out_d[s, t * 128:(t + 1) * 128, :], ot[k][:], reads=[R_ot[k]], writes=[R_out], nowaw=True)

            pipeline(len(tiles), [c_L, c_T])
            S.barrier()
        S.barrier()
        S.finish()
        if os.environ.get("MARKS"):
            for ln, c in S.marks:
                print("MARK line", ln, c)
        print("program: insts", S.n_inst, "waits", S.n_wait, {k: v for k, v in S.cnt.items()})
    return nc


def _consts():
    f32 = np.float32
    rows = T // 64
    row = np.repeat(np.arange(rows, dtype=f32), 64)
    col = np.tile(np.arange(64, dtype=f32), rows)
    n_freq = 16
    inv_freq = (np.float32(10000.0) ** (-np.arange(n_freq, dtype=f32) / np.float32(n_freq))).astype(f32)
    ang_r = row[:, None] * inv_freq[None, :]
    ang_c = col[:, None] * inv_freq[None, :]
    ang = np.concatenate([ang_r, ang_r, ang_c, ang_c], axis=-1).astype(f32)
    cos = np.cos(ang).astype(f32).T
    sin = np.sin(ang).astype(f32).T
    cosT = np.ascontiguousarray(np.concatenate([cos, cos], axis=0))
    sinT = np.ascontiguousarray(np.concatenate([sin, sin], axis=0))
    rm = np.zeros((128, 128), f32)
    for m in range(128):
        if (m % 32) < 16:
            rm[m + 16, m] = -1.0
        else:
            rm[m - 16, m] = 1.0
    p = np.arange(128)[:, None]
    r = np.arange(128)[None, :]
    bf = ml_dtypes.bfloat16
    return {
        "cosT": cosT, "sinT": sinT, "rmat": rm.astype(bf),
        "mask_le": np.where(p <= r, 0.0, -30000.0).astype(f32).astype(bf), "mask_ge": np.where(p >= r, 0.0, -30000.0).astype(f32).astype(bf),
        "ident_bf": np.eye(128, dtype=f32).astype(bf), "ident_f": np.eye(128, dtype=f32),
        "iota256": np.ascontiguousarray(np.broadcast_to(np.arange(256, dtype=f32)[None, :], (128, 256))),
        "iota_p": np.stack([np.arange(128, dtype=f32), np.arange(128, dtype=f32) + 128], axis=1),
        "tokhl": np.stack([np.broadcast_to(np.arange(128, dtype=f32)[:, None], (128, 16)),
                           np.broadcast_to(128.0 * np.arange(16, dtype=f32)[None, :], (128, 16))], axis=2).astype(bf),
    }


def make_in_maps(inputs):
    f = lambda a: np.ascontiguousarray(np.asarray(a, dtype=np.float32))
    x, c, ctx, c_ctx = f(inputs["x"]), f(inputs["c"]), f(inputs["ctx"]), f(inputs["c_ctx"])
    b_mod = f(inputs["b_mod"])[0]
    shared = {
        "w_mod": f(inputs["w_mod"])[0], "b_mod": b_mod[None, :],
        "bmodT": np.ascontiguousarray(b_mod[:2048].reshape(16, 128).T),
        "n1gT": np.ascontiguousarray(f(inputs["norm1_g"])[0].reshape(8, 128).T),
        "n2g": f(inputs["norm2_g"]), "fng": f(inputs["final_norm_g"])[None, :],
        "w_in": f(inputs["w_in"])[0], "sink": f(inputs["attn_sink"])[0].reshape(1, 16),
        "convwT": np.ascontiguousarray(f(inputs["conv_w"])[0].reshape(3, 8, 128).transpose(2, 1, 0)),
        "w_pa": f(inputs["w_proj_attn"])[0], "w_pc": f(inputs["w_proj_conv"])[0], "w_out": f(inputs["w_out"])[0],
        "w_r": f(inputs["w_router"])[0], "w_eg": f(inputs["w_exp_gate"])[0], "w_eu": f(inputs["w_exp_up"])[0],
        "w_ed": f(inputs["w_exp_down"])[0],
    }
    shared.update(_consts())
    maps = []
    for i in range(8):
        vecs = np.stack([c[2 * i], c[2 * i + 1], c_ctx, np.zeros_like(c_ctx)], axis=1)
        m = dict(shared)
        m["x"] = x[2 * i:2 * i + 2]
        m["ctx"] = ctx[2 * i:2 * i + 2]
        m["cvec"] = np.ascontiguousarray(vecs.reshape(8, 128, 4).transpose(1, 0, 2))
        maps.append(m)
    return maps


def kernel(**inputs):
    maps = make_in_maps(inputs)
    nc = build_program()
    res = run_bass_kernel_spmd(nc, maps, core_ids=list(range(8)))
    return np.concatenate([r["out"] for r in res.results], axis=0).astype(np.float32)
```

```python
import os
import numpy as np
import ml_dtypes
from contextlib import ExitStack
import concourse.bass as bass
import concourse.mybir as mybir
from concourse.bass_utils import run_bass_kernel_spmd

F32 = mybir.dt.float32
BF16 = mybir.dt.bfloat16
AF = mybir.ActivationFunctionType
ALU = mybir.AluOpType

D = 1024
T = 2048
L = 256
NS = 2
NE = 16
CAP = 256
EPS = 1e-6
IN_W = 6656
OFF_Q, OFF_K, OFF_V, OFF_U, OFF_B, OFF_C, OFF_GA, OFF_GC = 0, 1024, 1280, 1536, 2560, 3584, 4608, 5632

SEM_CH = 4000
STQ = "pool"
DMA_RING = 8


class Reg:
    __slots__ = ("name", "lw", "rd")

    def __init__(self, name=""):
        self.name = name
        self.lw = {}
        self.rd = []


class Ev:
    __slots__ = ("key", "seq", "sem", "val")

    def __init__(self, key, seq, sem, val):
        self.key, self.seq, self.sem, self.val = key, seq, sem, val


class Sched:
    def __init__(self, nc, stack):
        self.nc = nc
        self.stack = stack
        self.eng = {"pe": nc.tensor, "act": nc.scalar, "dve": nc.vector, "pool": nc.gpsimd, "sp": nc.sync}
        self.cnt = {k: 0 for k in self.eng}
        self.sems = {k: [] for k in self.eng}
        self.last = {k: None for k in self.eng}
        self.waited = {k: {} for k in self.eng}
        self.dq = {}
        self.same_engine_sync = {"act": True, "dve": True, "pool": True, "pe": False, "sp": False}
        self.n_wait = 0
        self.n_inst = 0
        self.marks = []
        self.marker = None

    def _sem(self, name):
        return self.stack.enter_context(self.nc.semaphore(name))

    def _eng_event(self, e):
        seq = self.cnt[e]
        self.cnt[e] += 1
        ci = seq // SEM_CH
        while len(self.sems[e]) <= ci:
            self.sems[e].append(self._sem(f"s_{e}_{len(self.sems[e])}"))
        ev = Ev(e, seq, self.sems[e][ci], seq % SEM_CH + 1)
        self.last[e] = ev
        return ev

    def _wait(self, e, ev):
        if ev is None:
            return
        if ev.key == e and not self.same_engine_sync[e]:
            return
        w = self.waited[e]
        if w.get(ev.key, -1) >= ev.seq:
            return
        self.eng[e].wait_ge(ev.sem, ev.val)
        self.n_wait += 1
        w[ev.key] = ev.seq

    def _deps(self, e, reads, writes, nowaw=False):
        need = {}

        def add(ev):
            o = need.get(ev.key)
            if o is None or o.seq < ev.seq:
                need[ev.key] = ev

        for r in reads:
            for ev in r.lw.values():
                add(ev)
        for wr in writes:
            if not nowaw:
                for ev in wr.lw.values():
                    add(ev)
            for ev in wr.rd:
                add(ev)
        for ev in need.values():
            self._wait(e, ev)

    def _commit(self, ev, reads, writes, nowaw=False):
        for r in reads:
            r.rd.append(ev)
            if len(r.rd) > 48:
                d = {}
                for x in r.rd:
                    if x.key not in d or d[x.key].seq < x.seq:
                        d[x.key] = x
                r.rd = list(d.values())
        for wr in writes:
            if nowaw:
                wr.lw[ev.key] = ev
            else:
                wr.lw = {ev.key: ev}
            wr.rd = []

    def op(self, e, fn, reads=(), writes=(), nowaw=False):
        self._deps(e, reads, writes, nowaw)
        ev = self._eng_event(e)
        fn(self.eng[e]).then_inc(ev.sem, 1)
        self.n_inst += 1
        self._commit(ev, reads, writes, nowaw)
        return ev

    def dma(self, q, out, in_, reads=(), writes=(), nowaw=False, **kw):
        st = self.dq.setdefault(q, {"n": 0, "sems": [], "evs": {}})
        n = st["n"]
        st["n"] += 1
        slot = n % DMA_RING
        while len(st["sems"]) <= slot:
            st["sems"].append(self._sem(f"d_{q}_{len(st['sems'])}"))
        self._wait(q, st["evs"].get(slot))
        self._deps(q, reads, writes, nowaw)
        ev = Ev(f"dma_{q}_{slot}", n // DMA_RING, st["sems"][slot], 16 * (n // DMA_RING + 1))
        self.eng[q].dma_start(out=out, in_=in_, **kw).then_inc(ev.sem, 16)
        st["evs"][slot] = ev
        self.n_inst += 1
        self._commit(ev, reads, writes, nowaw)
        return ev

    def dma_custom(self, q, issue, reads=(), writes=(), nowaw=False):
        st = self.dq.setdefault(q, {"n": 0, "sems": [], "evs": {}})
        n = st["n"]
        st["n"] += 1
        slot = n % DMA_RING
        while len(st["sems"]) <= slot:
            st["sems"].append(self._sem(f"d_{q}_{len(st['sems'])}"))
        self._wait(q, st["evs"].get(slot))
        self._deps(q, reads, writes, nowaw)
        ev = Ev(f"dma_{q}_{slot}", n // DMA_RING, st["sems"][slot], 16 * (n // DMA_RING + 1))
        issue(self.eng[q]).then_inc(ev.sem, 16)
        st["evs"][slot] = ev
        self.n_inst += 1
        self._commit(ev, reads, writes, nowaw)
        return ev

    def barrier(self):
        import sys as _sys
        self.marks.append((_sys._getframe(1).f_lineno, dict(self.cnt)))
        evs = [ev for ev in self.last.values() if ev is not None]
        for st in self.dq.values():
            evs.extend(st["evs"].values())
        for e in self.eng:
            for ev in evs:
                if ev.key != e:
                    self._wait(e, ev)
        if self.marker is not None:
            self.eng["pool"].memset(self.marker, float(len(self.marks)))

    def finish(self):
        evs = [ev for ev in self.last.values() if ev is not None]
        for st in self.dq.values():
            evs.extend(st["evs"].values())
        for ev in evs:
            if ev.key != "sp":
                self._wait("sp", ev)


def build_program(dbg=None, stop_after=None, skip_pre=False):
    nc = bass.Bass("TRN2", target_bir_lowering=False)

    def din(name, shape, dt=F32):
        return nc.dram_tensor(name, list(shape), dt, kind="ExternalInput").ap()

    def dscr(name, shape, dt):
        return nc.dram_tensor(name, list(shape), dt, kind="Internal").ap()

    x_d = din("x", [NS, T, D])
    ctx_d = din("ctx", [NS, L, D])
    cvec_d = din("cvec", [128, 8, 4])
    wmod_d = din("w_mod", [D, 6 * D])
    bmod_d = din("b_mod", [1, 6 * D])
    bmodT_d = din("bmodT", [128, 16])
    n1gT_d = din("n1gT", [128, 8])
    n2g_d = din("n2g", [1, D])
    fng_d = din("fng", [1, D])
    win_d = din("w_in", [D, IN_W])
    sink_d = din("sink", [1, 16])
    convw_d = din("convwT", [128, 8, 3])
    wpa_d = din("w_pa", [D, D])
    wpc_d = din("w_pc", [D, D])
    wout_d = din("w_out", [D, D])
    wr_d = din("w_r", [D, NE])
    weg_d = din("w_eg", [NE, D, D])
    weu_d = din("w_eu", [NE, D, D])
    wed_d = din("w_ed", [NE, D, D])
    cosT_d = din("cosT", [128, T])
    sinT_d = din("sinT", [128, T])
    rmat_d = din("rmat", [128, 128], BF16)
    mle_d = din("mask_le", [128, 128], BF16)
    mge_d = din("mask_ge", [128, 128], BF16)
    identb_d = din("ident_bf", [128, 128], BF16)
    identf_d = din("ident_f", [128, 128])
    iota_d = din("iota256", [128, 256])
    iotap_d = din("iota_p", [128, 2])
    tokhl_d = din("tokhl", [128, 16, 2], BF16)
    out_d = nc.dram_tensor("out", [NS, T, D], F32, kind="ExternalOutput").ap()
    dbg_d = {}
    if dbg:
        for name, shape in dbg.items():
            dbg_d[name] = nc.dram_tensor("dbg_" + name, list(shape), F32, kind="ExternalOutput").ap()

    modrows_d = dscr("modrows", [4, 6 * D], F32)
    md_d = dscr("md", [NS, 8, 128, T], BF16)
    x1_d = dscr("x1", [NS, T, D], F32)
    hx2_d = dscr("hx2", [NS, T, D], BF16)
    posr_d = dscr("posr", [NS, NE, T], BF16)
    ye_d = dscr("ye", [NS, NE, CAP, D], BF16)

    with ExitStack() as top:
        S = Sched(nc, top)

        uid = [0]

        def sb(st, name, shape, dt):
            uid[0] += 1
            return st.enter_context(nc.sbuf_tensor(f"sb{uid[0]}_{name}", list(shape), dt))

        def ps(st, name, shape, dt=F32):
            uid[0] += 1
            return st.enter_context(nc.psum_tensor(f"ps{uid[0]}_{name}", list(shape), dt))

        def mm(out, lhsT, rhs, start, stop, reads, writes):
            S.op("pe", lambda e: e.matmul(out, lhsT=lhsT, rhs=rhs, start=start, stop=stop), reads, writes)

        def tr(out, in_, ident, reads, writes):
            S.op("pe", lambda e: e.transpose(out, in_, ident), reads, writes)

        def act(out, in_, func, reads, writes, **kw):
            S.op("act", lambda e: e.activation(out=out, in_=in_, func=func, **kw), reads, writes)

        def tt(eng, out, in0, in1, op, reads, writes):
            S.op(eng, lambda e: e.tensor_tensor(out=out, in0=in0, in1=in1, op=op), reads, writes)

        def ts(eng, out, in0, s1, s2, op0, op1, reads, writes):
            if op1 is None:
                S.op(eng, lambda e: e.tensor_scalar(out=out, in0=in0, scalar1=s1, scalar2=None, op0=op0), reads, writes)
            else:
                S.op(eng, lambda e: e.tensor_scalar(out=out, in0=in0, scalar1=s1, scalar2=s2, op0=op0, op1=op1), reads, writes)

        def stt(out, in0, scalar, in1, op0, op1, reads, writes):
            S.op("dve", lambda e: e.scalar_tensor_tensor(out=out, in0=in0, scalar=scalar, in1=in1, op0=op0, op1=op1), reads, writes)

        def cp(eng, out, in_, reads, writes, nowaw=False):
            if eng == "act":
                S.op("act", lambda e: e.activation(out=out, in_=in_, func=AF.Copy), reads, writes, nowaw)
            else:
                S.op(eng, lambda e: e.tensor_copy(out=out, in_=in_), reads, writes, nowaw)

        def pipeline(n, stages, skew=1):
            for step in range(n + (len(stages) - 1) * skew):
                for k, f in enumerate(stages):
                    i = step - k * skew
                    if 0 <= i < n:
                        f(i)

        def dbg_dump(name, src_ap, reads, view=None):
            if name in dbg_d:
                dst = dbg_d[name] if view is None else view(dbg_d[name])
                S.dma("pool", dst, src_ap, reads=reads, writes=[Reg()])

        identb = sb(top, "identb", [128, 128], BF16)
        identf = sb(top, "identf", [128, 128], F32)
        rmat = sb(top, "rmat", [128, 128], BF16)
        mle = sb(top, "mle", [128, 128], BF16)
        mge = sb(top, "mge", [128, 128], BF16)
        iota = sb(top, "iota", [128, 256], F32)
        iotap = sb(top, "iotap", [128, 2], F32)
        convw = sb(top, "convw", [128, 8, 3], F32)
        n1gT = sb(top, "n1gT_s", [128, 8], F32)
        bmodT = sb(top, "bmodT_s", [128, 16], F32)
        esink = sb(top, "esink", [128, 16], F32)
        modT = sb(top, "modT", [128, 16, 4], F32)
        A1s = sb(top, "A1s", [128, 8, 4], F32)
        wr_f = sb(top, "wr_f", [128, 8, 128], F32)
        aff = sb(top, "aff", [64, T], F32)
        ones16 = sb(top, "ones16", [64, 64], F32)
        epsc = sb(top, "epsc", [128, 1], F32)
        R_c = Reg("consts")
        if os.environ.get("MARKS"):
            mark_t = sb(top, "mark_t", [1, 8], F32)
            S.marker = mark_t[0:1, 0:1]
        for dst, src in ((identb, identb_d), (identf, identf_d), (rmat, rmat_d), (mle, mle_d), (mge, mge_d),
                         (iota, iota_d), (iotap, iotap_d), (convw, convw_d), (n1gT, n1gT_d), (bmodT, bmodT_d)):
            S.dma("sp", dst[:], src, writes=[R_c], nowaw=True)
        S.dma("sp", esink[:], sink_d.to_broadcast([128, 16]), writes=[R_c], nowaw=True)
        S.op("dve", lambda e: e.memset(wr_f[:], 0.0), writes=[R_c])
        S.op("dve", lambda e: e.memset(aff[:], 0.0), writes=[R_c])
        S.op("dve", lambda e: e.memset(ones16[:], 0.0), writes=[R_c])
        S.op("dve", lambda e: e.memset(epsc[:], EPS), writes=[R_c])
        S.op("dve", lambda e: e.memset(ones16[0:16, 0:16], 1.0), writes=[R_c])
        S.op("dve", lambda e: e.memset(ones16[32:48, 32:48], 1.0), writes=[R_c])
        wrv = wr_d.rearrange("(k p) e -> p k e", p=128)
        S.dma("sp", wr_f[:, :, 0:16], wrv, writes=[R_c])
        S.dma("sp", wr_f[:, :, 32:48], wrv, writes=[R_c])
        act(esink[:], esink[:], AF.Exp, [R_c], [R_c])
        S.barrier()

        R_modT = Reg()
        R_modrows = Reg()
        with ExitStack() as st:
            cv = sb(st, "cv", [128, 8, 4], F32)
            scv = sb(st, "scv", [128, 8, 4], F32)
            bm3 = sb(st, "bm3", [4, 6 * D], F32)
            wmb = [sb(st, f"wmb{i}", [128, 8, 512], F32) for i in range(3)]
            mrow = [sb(st, f"mrow{i}", [4, 512], F32) for i in range(4)]
            tmp1 = sb(st, "tmp1", [128, 8, 4], F32)
            ps_r = [ps(st, f"ps_r{i}", [128, 512]) for i in range(2)]
            ps_f = [ps(st, f"ps_f{i}", [128, 512]) for i in range(2)]
            R_cv, R_bm3 = Reg(), Reg()
            R_wmb = [Reg(), Reg(), Reg()]
            R_mrow = [Reg() for _ in range(4)]
            R_psr = [Reg(), Reg()]
            R_psf = [Reg(), Reg()]
            R_modT = Reg()
            R_modrows = Reg()
            S.dma("sp", cv[:], cvec_d, writes=[R_cv])
            S.dma("sp", bm3[:], bmod_d.to_broadcast([4, 6 * D]), writes=[R_bm3])
            act(scv[:], cv[:], AF.Silu, [R_cv], [R_cv])
            wmv = wmod_d.rearrange("(k p) n -> p k n", p=128)
            nf = 0
            for b in range(12):
                w = wmb[b % 3]
                Rw = R_wmb[b % 3]
                S.dma("sp", w[:], wmv[:, :, b * 512:(b + 1) * 512], writes=[Rw])
                pr = ps_r[b % 2]
                mr, Rm = mrow[b % 4], R_mrow[b % 4]
                for k in range(8):
                    mm(pr[0:4, :], scv[:, k, :], w[:, k, :], k == 0, k == 7, [R_cv, Rw], [R_psr[b % 2]])
                tt("dve", mr[:], pr[0:4, :], bm3[:, b * 512:(b + 1) * 512], ALU.add, [R_psr[b % 2], R_bm3], [Rm])
                if b >= 4:
                    S.dma("sp", modrows_d[:, b * 512:(b + 1) * 512], mr[:], reads=[Rm], writes=[R_modrows], nowaw=True)
                else:
                    pf = ps_f[b % 2]
                    for j in range(4):
                        tr(pf[:, j * 4:(j + 1) * 4], mr[0:4, j * 128:(j + 1) * 128], identf[0:4, 0:4], [Rm, R_c], [R_psf[b % 2]])
                    cp("dve", modT[:, b * 4:(b + 1) * 4, :], pf[:, 0:16].rearrange("p (j v) -> p j v", v=4), [R_psf[b % 2]], [R_modT])
            ts("dve", tmp1[:], modT[:, 8:16, :], 1.0, None, ALU.add, None, [R_modT], [R_cv])
            for j in range(4):
                tt("dve", A1s[:, :, j], tmp1[:, :, j], n1gT[:], ALU.mult, [R_cv, R_c], [R_modT])
            S.barrier()
        if dbg:
            dbg_dump("modT", modT[:], [R_modT])
            dbg_dump("A1s", A1s[:], [R_modT])

        if stop_after == "P0":
            S.barrier(); S.finish(); return nc

        cast_rr = [0]

        def load_w(st_tiles, R_st, dst_ap, pieces, R_dst, ncols):
            i = cast_rr[0]
            cast_rr[0] += 1
            stg = st_tiles[i % len(st_tiles)]
            Rs = R_st[i % len(st_tiles)]
            for (c0, src, wd) in pieces:
                S.dma("sp", stg[:, :, c0:c0 + wd], src, writes=[Rs], nowaw=True)
            eng = ("act", "act", "dve")[i % 3]
            cp(eng, dst_ap, stg[:, :, 0:ncols], [Rs], [R_dst], nowaw=True)

        winv = win_d.rearrange("(k p) n -> p k n", p=128)

        def rms_rstd(st_name, xt, R_x, junk, R_junk, ssq, rstd, R_stat, recip=True):
            act(junk, xt, AF.Square, [R_x], [R_junk, R_stat], accum_out=ssq)
            act(ssq, ssq, AF.Sqrt, [R_stat], [R_stat], scale=1.0 / D, bias=epsc[:, 0:1])
            if recip:
                S.op("dve", lambda e: e.reciprocal(out=rstd, in_=ssq), [R_stat], [R_stat])

        R_aff = Reg("aff")
        R_x1d, R_hx2d = Reg(), Reg()

        for s in range(0 if skip_pre else NS):
            with ExitStack() as sa:
                H = sb(sa, "H", [128, 8, T], BF16)
                R_H = [[Reg() for _ in range(4)] for _ in range(8)]
                QO = sb(sa, "QO", [128, 8, T], BF16)
                R_Q = [[Reg() for _ in range(16)] for _ in range(8)]
                stg = [sb(sa, f"stg{i}", [128, 8, 256], F32) for i in range(3)]
                R_stg = [Reg() for _ in range(3)]
                wring = [sb(sa, f"wring{i}", [128, 8, 256], BF16) for i in range(4)]
                R_wr = [Reg() for _ in range(4)]
                wi = [0]

                def next_w(pieces, ncols=256, ring=None):
                    tiles, regs, cnt = ring if ring is not None else (wring, R_wr, wi)
                    i = cnt[0] % len(tiles)
                    cnt[0] += 1
                    load_w(stg, R_stg, tiles[i][:, :, 0:ncols], pieces, regs[i], ncols)
                    return tiles[i], regs[i]

                with ExitStack() as st:
                    KtA = sb(st, "KtA", [128, 4, T + L], BF16)
                    KtB = sb(st, "KtB", [128, 4, T + L], BF16)
                    Kt2 = [KtA, KtB]
                    R_K = [[Reg() for _ in range(18)] for _ in range(4)]
                    S.op("pool", lambda e: e.memset(KtA[64:128, :, :], 0.0), writes=[r for rr in R_K for r in rr])
                    S.op("pool", lambda e: e.memset(KtB[0:64, :, :], 0.0), writes=[r for rr in R_K for r in rr], nowaw=True)
                    V = sb(st, "V", [128, 18, 4, 65], BF16)
                    R_V = [Reg() for _ in range(18)]
                    Hc = sb(st, "Hc", [128, 8, L], BF16)
                    R_Hc = Reg()
                    R_cs = Reg()
                    S.op("pool", lambda e: e.memset(V[:, :, :, 64:65], 1.0), writes=R_V)

                    with ExitStack() as s1:
                        xt = [sb(s1, f"xt{i}", [128, D], F32) for i in range(2)]
                        xn = [sb(s1, f"xn{i}", [128, D], F32) for i in range(2)]
                        junk = sb(s1, "junk1", [128, D], F32)
                        stat = [sb(s1, f"stat{i}", [128, 2], F32) for i in range(2)]
                        pst = [ps(s1, f"pst{i}", [128, 8, 128]) for i in range(2)]
                        R_xt, R_xn, R_st1 = [Reg(), Reg()], [Reg(), Reg()], [Reg(), Reg()]
                        R_pst = [[Reg(), Reg()], [Reg(), Reg()]]
                        R_junk = Reg()
                        def a1_L(t):
                            b = t % 2
                            src = x_d[s, t * 128:(t + 1) * 128, :] if t < 16 else ctx_d[s, (t - 16) * 128:(t - 15) * 128, :]
                            S.dma("sp", xt[b][:], src, writes=[R_xt[b]])
                            rms_rstd("a1", xt[b][:], R_xt[b], junk[:], R_junk, stat[b][:, 0:1], stat[b][:, 1:2], R_st1[b])
                            ts("dve", xn[b][:], xt[b][:], stat[b][:, 1:2], None, ALU.mult, None, [R_xt[b], R_st1[b]], [R_xn[b]])

                        def a1_X(t):
                            b = t % 2
                            mj = s if t < 16 else 2
                            for k in range(8):
                                tr(pst[b][:, k, :], xn[b][:, k * 128:(k + 1) * 128], identf[:], [R_xn[b], R_c], [R_pst[b][k // 4]])
                            for k in range(8):
                                if t < 16:
                                    dst = H[:, k, t * 128:(t + 1) * 128]
                                    Rd = [R_H[k][t // 4]]
                                else:
                                    dst = Hc[:, k, (t - 16) * 128:(t - 15) * 128]
                                    Rd = [R_Hc]
                                if k < 4:
                                    act(dst, pst[b][:, k, :], AF.Identity, [R_pst[b][0], R_modT], Rd,
                                        scale=A1s[:, k, mj:mj + 1], bias=modT[:, k, mj:mj + 1])
                                else:
                                    ts("dve", dst, pst[b][:, k, :], A1s[:, k, mj:mj + 1], modT[:, k, mj:mj + 1],
                                       ALU.mult, ALU.add, [R_pst[b][1], R_modT], Rd)

                        pipeline(18, [a1_L, a1_X])
                        S.barrier()
                    if stop_after == "A1":
                        S.barrier(); S.finish(); return nc

                    with ExitStack() as s2:
                        cosT = sb(s2, "cosT", [128, T], F32)
                        sinT = sb(s2, "sinT", [128, T], F32)
                        S.dma("sp", cosT[:], cosT_d, writes=[R_cs], nowaw=True)
                        S.dma("sp", sinT[:], sinT_d, writes=[R_cs], nowaw=True)
                        psq = [ps(s2, f"psq{i}", [128, 512]) for i in range(3)]
                        psr2 = [ps(s2, f"psr2{i}", [128, 512]) for i in range(3)]
                        R_psq = [Reg() for _ in range(3)]
                        R_psr2 = [Reg() for _ in range(3)]
                        qb = [sb(s2, f"qb{i}", [128, 512], BF16) for i in range(3)]
                        t1 = [sb(s2, f"t1{i}", [128, 512], F32) for i in range(3)]
                        t2 = [sb(s2, f"t2{i}", [128, 512], F32) for i in range(3)]
                        R_qb, R_t1, R_t2 = [Reg() for _ in range(3)], [Reg() for _ in range(3)], [Reg() for _ in range(3)]
                        u = [0]

                        pend = [None]

                        def rope_R(i, dst, R_dst, g):
                            mm(psr2[i][:], rmat[:], qb[i][:], True, True, [R_qb[i], R_c], [R_psr2[i]])
                            tt("dve", t1[i][:], psq[i][:], cosT[:, g * 512:(g + 1) * 512], ALU.mult, [R_psq[i], R_cs, R_qb[i]], [R_t1[i]])
                            tt("dve", t2[i][:], psr2[i][:], sinT[:, g * 512:(g + 1) * 512], ALU.mult, [R_psr2[i], R_cs], [R_t2[i]])
                            if isinstance(dst, tuple):
                                tt("dve", dst[0], t1[i][0:64, :], t2[i][0:64, :], ALU.add, [R_t1[i], R_t2[i]], R_dst, )
                                S.op("dve", lambda e: e.tensor_tensor(out=dst[1], in0=t1[i][64:128, :], in1=t2[i][64:128, :], op=ALU.add),
                                     [R_t1[i], R_t2[i]], R_dst, nowaw=True)
                            else:
                                tt("dve", dst, t1[i][:], t2[i][:], ALU.add, [R_t1[i], R_t2[i]], R_dst)

                        def flush_pend():
                            if pend[0] is not None:
                                rope_R(*pend[0])
                                pend[0] = None

                        def rope_unit(wt, Rw, col0, Hsrc, R_Hs, tok0, ntok, dst, R_dst, rope, g):
                            i = u[0] % 3
                            u[0] += 1
                            for k in range(8):
                                mm(psq[i][:, 0:ntok], wt[:, k, col0:col0 + 128], Hsrc[:, k, tok0:tok0 + ntok], k == 0, k == 7,
                                   [Rw] + R_Hs, [R_psq[i]])
                            if not rope:
                                if isinstance(dst, tuple):
                                    act(dst[0], psq[i][0:64, 0:ntok], AF.Copy, [R_psq[i]], R_dst)
                                    S.op("act", lambda e: e.activation(out=dst[1], in_=psq[i][64:128, 0:ntok], func=AF.Copy), [R_psq[i]], R_dst, nowaw=True)
                                else:
                                    act(dst, psq[i][:, 0:ntok], AF.Copy, [R_psq[i]], R_dst)
                                flush_pend()
                                return
                            act(qb[i][:], psq[i][:], AF.Copy, [R_psq[i]], [R_qb[i]])
                            flush_pend()
                            pend[0] = (i, dst, R_dst, g)

                        def a2_pieces(b_):
                            if b_ < 4:
                                return [(0, winv[:, :, OFF_Q + b_ * 256:OFF_Q + (b_ + 1) * 256], 256)]
                            if b_ < 6:
                                pcs = []
                                for hh_ in range(2):
                                    kv_ = (b_ - 4) * 2 + hh_
                                    srcw_ = winv[:, :, OFF_K + kv_ * 64:OFF_K + (kv_ + 1) * 64]
                                    pcs.append((hh_ * 128, srcw_, 64))
                                    pcs.append((hh_ * 128 + 64, srcw_, 64))
                                return pcs
                            return [(0, winv[:, :, OFF_V:OFF_V + 256], 256)]

                        a2w = {}

                        def a2_get(b_):
                            for bb in (b_, b_ + 1, b_ + 2):
                                if bb < 7 and bb not in a2w:
                                    a2w[bb] = next_w(a2_pieces(bb))
                            return a2w[b_]

                        for blk in range(4):
                            wt, Rw = a2_get(blk)
                            for cc in range(2):
                                c = blk * 2 + cc
                                for g in range(4):
                                    rope_unit(wt, Rw, cc * 128, H, [R_H[k][g] for k in range(8)], g * 512, 512,
                                              QO[:, c, g * 512:(g + 1) * 512], [R_Q[c][g * 4 + j] for j in range(4)], True, g)
                        for blk in range(2):
                            wt, Rw = a2_get(4 + blk)
                            for hh in range(2):
                                kv = blk * 2 + hh
                                for g in range(4):
                                    rope_unit(wt, Rw, hh * 128, H, [R_H[k][g] for k in range(8)], g * 512, 512,
                                              (KtA[0:64, kv, g * 512:(g + 1) * 512], KtB[64:128, kv, g * 512:(g + 1) * 512]), [R_K[kv][g * 4 + j] for j in range(4)], True, g)
                                rope_unit(wt, Rw, hh * 128, Hc, [R_Hc], 0, L, (KtA[0:64, kv, T:T + L], KtB[64:128, kv, T:T + L]), [R_K[kv][16], R_K[kv][17]], False, 0)
                        flush_pend()
                        wt, Rw = a2_get(6)
                        for t in range(18):
                            i = u[0] % 3
                            u[0] += 1
                            for k in range(8):
                                lhs = H[:, k, t * 128:(t + 1) * 128] if t < 16 else Hc[:, k, (t - 16) * 128:(t - 15) * 128]
                                Rl = [R_H[k][t // 4]] if t < 16 else [R_Hc]
                                mm(psq[i][:, 0:256], lhs, wt[:, k, 0:256], k == 0, k == 7, Rl + [Rw], [R_psq[i]])
                            cp("act" if t % 2 else "dve", V[:, t, :, 0:64], psq[i][:, 0:256].rearrange("p (g d) -> p g d", g=4),
                               [R_psq[i]], [R_V[t]])
                        S.barrier()
                    if dbg and s == 0:
                        dbg_dump("H", H[:], [r for rr in R_H for r in rr])
                        dbg_dump("Q", QO[:], [r for rr in R_Q for r in rr])
                        dbg_dump("Kt", KtA[:], [r for rr in R_K for r in rr])
                        dbg_dump("V", V[:], R_V)
                    if stop_after == "A2":
                        S.barrier()
                        S.finish()
                        return nc

                    with ExitStack() as s3:
                        pss = [ps(s3, f"pss{i}", [128, 512]) for i in range(4)]
                        R_pss = [Reg() for _ in range(4)]
                        pso = [ps(s3, f"pso{i}", [128, 4, 128]) for i in range(2)]
                        R_pso = [Reg() for _ in range(2)]
                        pstr = [ps(s3, f"pstr{i}", [128, 1024], BF16) for i in range(2)]
                        R_pstr = [Reg() for _ in range(2)]
                        PT = [[sb(s3, f"PT{hh}_{i}", [128, 384], BF16) for i in range(4)] for hh in range(2)]
                        R_PT = [[Reg() for _ in range(4)] for _ in range(2)]
                        PTc = [[[sb(s3, f"PTc{par}_{hh}_{cb}", [128, T], BF16) for cb in range(2)] for hh in range(2)] for par in range(2)]
                        R_PTc = [[[[Reg() for _ in range(4)] for _ in range(2)] for _ in range(2)] for _ in range(2)]
                        den = [sb(s3, f"den{i}", [128, 4], F32) for i in range(2)]
                        R_den = [Reg(), Reg()]
                        ob = [sb(s3, f"ob{i}", [128, 128], BF16) for i in range(2)]
                        R_ob = [Reg(), Reg()]
                        n_s = [0]

                        def ctx_unit(c, idx):
                            hh, rem = divmod(idx, 8)
                            cb, g = divmod(rem, 4)
                            kv = c // 2
                            p0 = hh * 64
                            i = n_s[0] % 4
                            n_s[0] += 1
                            mm(pss[i][:], Kt2[hh][:, kv, T + cb * 128:T + (cb + 1) * 128],
                               QO[:, c, g * 512:(g + 1) * 512], True, True,
                               [R_K[kv][16 + cb]] + [R_Q[c][g * 4 + j] for j in range(4)], [R_pss[i]])
                            act(PTc[c % 2][hh][cb][:, g * 512:(g + 1) * 512], pss[i][:], AF.Exp, [R_pss[i]],
                                [R_PTc[c % 2][hh][cb][g]], scale=0.125)

                        def score_unit(c, j):
                            kv = c // 2
                            qlo, qhi = max(j - 1, 0), min(j + 1, 15)
                            n = (qhi - qlo + 1) * 128
                            for hh in range(2):
                                p0 = hh * 64
                                i = n_s[0] % 4
                                n_s[0] += 1
                                nmask = (1 if j >= 1 else 0) + (1 if j <= 14 else 0)
                                mm(pss[i][:, 0:n], Kt2[hh][:, kv, j * 128:(j + 1) * 128], QO[:, c, qlo * 128:(qhi + 1) * 128],
                                   True, nmask == 0, [R_K[kv][j]] + [R_Q[c][q] for q in range(qlo, qhi + 1)], [R_pss[i]])
                                km = 0
                                if j >= 1:
                                    km += 1
                                    mm(pss[i][:, 0:128], identb[:], mle[:], False, km == nmask, [R_c], [R_pss[i]])
                                if j <= 14:
                                    km += 1
                                    mm(pss[i][:, n - 128:n], identb[:], mge[:], False, km == nmask, [R_c], [R_pss[i]])
                                act(PT[hh][j % 4][:, 0:n], pss[i][:, 0:n], AF.Exp, [R_pss[i]], [R_PT[hh][j % 4]], scale=0.125)

                        def pv_mm(c, iq):
                            kv = c // 2
                            io = iq % 2
                            for hh in range(2):
                                jl = [j for j in (iq - 1, iq, iq + 1) if 0 <= j < 16]
                                nmm = len(jl) + 2
                                cnt = 0
                                for j in jl:
                                    qlo = max(j - 1, 0)
                                    off = (iq - qlo) * 128
                                    cnt += 1
                                    mm(pso[io][:, hh, 0:65], PT[hh][j % 4][:, off:off + 128], V[:, j, kv, :], cnt == 1, cnt == nmm,
                                       [R_PT[hh][j % 4], R_V[j]], [R_pso[io]])
                                for cb in range(2):
                                    cnt += 1
                                    mm(pso[io][:, hh, 0:65], PTc[c % 2][hh][cb][:, iq * 128:(iq + 1) * 128], V[:, 16 + cb, kv, :], cnt == 1, cnt == nmm,
                                       [R_PTc[c % 2][hh][cb][iq // 4], R_V[16 + cb]], [R_pso[io]])
                            d = den[io]
                            tt("dve", d[:, 0:2], pso[io][:, 0:2, 64], esink[:, 2 * c:2 * c + 2], ALU.add, [R_pso[io], R_c], [R_den[io]])
                            S.op("dve", lambda e: e.reciprocal(out=d[:, 2:4], in_=d[:, 0:2]), [R_den[io]], [R_den[io]])
                            for hh in range(2):
                                ts("dve", ob[io][:, hh * 64:(hh + 1) * 64], pso[io][:, hh, 0:64], d[:, 2 + hh:3 + hh], None, ALU.mult, None,
                                   [R_pso[io], R_den[io]], [R_ob[io]])

                        def pv_fin(c, iq):
                            io = iq % 2
                            tr(pstr[io][:, 0:128], ob[io][:], identb[:], [R_ob[io], R_c], [R_pstr[io]])
                            cp("act" if iq % 2 else "dve", QO[:, c, iq * 128:(iq + 1) * 128], pstr[io][:, 0:128], [R_pstr[io]], [R_Q[c][iq]])

                        for idx in range(16):
                            ctx_unit(0, idx)
                        NU = 8 * 16
                        for u in range(NU + 3):
                            if u < NU:
                                c, j = divmod(u, 16)
                                score_unit(c, j)
                                if c + 1 < 8:
                                    ctx_unit(c + 1, j)
                            if 0 <= u - 2 < NU:
                                pv_mm(*divmod(u - 2, 16))
                            if 0 <= u - 3 < NU:
                                pv_fin(*divmod(u - 3, 16))
                        S.barrier()
                if dbg and s == 0:
                    dbg_dump("O", QO[:], [r for rr in R_Q for r in rr])
                if stop_after == "A3":
                    S.barrier()
                    S.finish()
                    return nc

                Y = sb(sa, "Y", [128, 8, T], BF16)
                R_Y = [Reg() for _ in range(8)]
                with ExitStack() as s4:
                    psu = [ps(s4, f"psu{i}", [128, 512]) for i in range(6)]
                    R_psu = [Reg() for _ in range(6)]
                    zb = [sb(s4, f"zb{i}", [128, T + 2], F32) for i in range(2)]
                    R_zb = [Reg(), Reg()]
                    Bs = [sb(s4, f"Bs{i}", [128, T], F32) for i in range(2)]
                    R_Bs = [Reg(), Reg()]
                    us = [sb(s4, f"us{i}", [128, 512], F32) for i in range(2)]
                    R_us = [Reg(), Reg()]
                    acc = sb(s4, "acc", [128, T], F32)
                    R_acc = Reg()
                    for i in range(2):
                        S.op("pool", lambda e, i=i: e.memset(zb[i][:, 0:1], 0.0), writes=[R_zb[i]])
                        S.op("pool", lambda e, i=i: e.memset(zb[i][:, T + 1:T + 2], 0.0), writes=[R_zb[i]])
                    n_u = 0
                    ring4 = (wring + [sb(s4, f"wr4_{i}", [128, 8, 256], BF16) for i in range(2)], R_wr + [Reg() for _ in range(2)], [0])

                    def load4(blk):
                        return [next_w([(0, winv[:, :, off + blk * 256:off + (blk + 1) * 256], 256)], ring=ring4) for off in (OFF_U, OFF_C, OFF_B)]

                    nxt4 = load4(0)
                    for blk in range(4):
                        wts = nxt4
                        if blk + 1 < 4:
                            nxt4 = load4(blk + 1)
                        for cc in range(2):
                            c = blk * 2 + cc
                            z = zb[c % 2]
                            Rz = R_zb[c % 2]
                            Bt = Bs[c % 2]
                            RB = R_Bs[c % 2]
                            for g in range(4):
                                pp = []
                                for (wt, Rw) in wts:
                                    i = n_u % 6
                                    n_u += 1
                                    for k in range(8):
                                        mm(psu[i][:], wt[:, k, cc * 128:(cc + 1) * 128], H[:, k, g * 512:(g + 1) * 512], k == 0, k == 7,
                                           [Rw, R_H[k][g]], [R_psu[i]])
                                    pp.append(i)
                                ui = (c * 4 + g) % 2
                                cp("act", us[ui][:], psu[pp[0]][:], [R_psu[pp[0]]], [R_us[ui]])
                                tt("dve", z[:, 1 + g * 512:1 + (g + 1) * 512], psu[pp[1]][:], us[ui][:], ALU.mult,
                                   [R_psu[pp[1]], R_us[ui]], [Rz])
                                cp("act", Bt[:, g * 512:(g + 1) * 512], psu[pp[2]][:], [R_psu[pp[2]]], [RB])
                            ts("dve", acc[:], z[:, 1:T + 1], convw[:, c, 1:2], None, ALU.mult, None, [Rz, R_c], [R_acc])
                            stt(acc[:], z[:, 0:T], convw[:, c, 0:1], acc[:], ALU.mult, ALU.add, [Rz, R_c, R_acc], [R_acc])
                            stt(acc[:], z[:, 2:T + 2], convw[:, c, 2:3], acc[:], ALU.mult, ALU.add, [Rz, R_c, R_acc], [R_acc])
                            tt("dve", Y[:, c, :], acc[:], Bt[:], ALU.mult, [R_acc, RB], [R_Y[c]])
                    S.barrier()
                if dbg and s == 0:
                    dbg_dump("Y", Y[:], R_Y)
                if stop_after == "A4":
                    S.barrier()
                    S.finish()
                    return nc

                R_md = [[Reg() for _ in range(4)] for _ in range(8)]
                with ExitStack() as s5:
                    psm = [ps(s5, f"psm{i}", [128, 512]) for i in range(8)]
                    R_psm = [Reg() for _ in range(8)]
                    sg = [sb(s5, f"sg{i}", [128, 512], F32) for i in range(4)]
                    R_sg = [Reg() for _ in range(4)]
                    tm = [sb(s5, f"tm{i}", [128, 512], F32) for i in range(4)]
                    R_tm = [Reg() for _ in range(4)]
                    mst = [sb(s5, f"mst{i}", [128, 512], BF16) for i in range(2)]
                    R_mst = [Reg(), Reg()]
                    wpav = wpa_d.rearrange("(k p) n -> p k n", p=128)
                    wpcv = wpc_d.rearrange("(k p) n -> p k n", p=128)
                    n_m = 0
                    n_g = 0
                    for blk in range(4):
                        if blk == 0:
                            ring5 = (wring + [sb(s5, f"wr5_{i}", [128, 8, 256], BF16) for i in range(4)], R_wr + [Reg() for _ in range(4)], [0])

                            def load5(b_):
                                return [
                                    (next_w([(0, winv[:, :, OFF_GA + b_ * 256:OFF_GA + (b_ + 1) * 256], 256)], ring=ring5), H, R_H, None),
                                    (next_w([(0, wpav[:, :, b_ * 256:(b_ + 1) * 256], 256)], ring=ring5), QO, None, R_Q),
                                    (next_w([(0, winv[:, :, OFF_GC + b_ * 256:OFF_GC + (b_ + 1) * 256], 256)], ring=ring5), H, R_H, None),
                                    (next_w([(0, wpcv[:, :, b_ * 256:(b_ + 1) * 256], 256)], ring=ring5), Y, None, None),
                                ]
                            nxt5 = load5(0)
                        wts = nxt5
                        if blk + 1 < 4:
                            nxt5 = load5(blk + 1)
                        for cc in range(2):
                            m = blk * 2 + cc
                            for g in range(4):
                                pp = []
                                for wi_, ((wt, Rw), src, RH_, RQ_) in enumerate(wts):
                                    i = n_m % 8
                                    n_m += 1
                                    for k in range(8):
                                        if RH_ is not None:
                                            rr = [RH_[k][g]]
                                        elif RQ_ is not None:
                                            rr = [RQ_[k][g * 4 + j] for j in range(4)]
                                        else:
                                            rr = [R_Y[k]]
                                        mm(psm[i][:], wt[:, k, cc * 128:(cc + 1) * 128], src[:, k, g * 512:(g + 1) * 512], k == 0, k == 7,
                                           [Rw] + rr, [R_psm[i]])
                                    pp.append(i)
                                a0, a1 = n_g % 4, (n_g + 1) % 4
                                n_g += 2
                                act(sg[a0][:], psm[pp[0]][:], AF.Sigmoid, [R_psm[pp[0]]], [R_sg[a0]])
                                act(sg[a1][:], psm[pp[2]][:], AF.Sigmoid, [R_psm[pp[2]]], [R_sg[a1]])
                                tt("dve", tm[a0][:], psm[pp[1]][:], sg[a0][:], ALU.mult, [R_psm[pp[1]], R_sg[a0]], [R_tm[a0]])
                                tt("dve", tm[a1][:], psm[pp[3]][:], sg[a1][:], ALU.mult, [R_psm[pp[3]], R_sg[a1]], [R_tm[a1]])
                                mi = (m * 4 + g) % 2
                                tt("dve", mst[mi][:], tm[a0][:], tm[a1][:], ALU.add, [R_tm[a0], R_tm[a1]], [R_mst[mi]])
                                S.dma(STQ, md_d[s, m, :, g * 512:(g + 1) * 512], mst[mi][:], reads=[R_mst[mi]], writes=[R_md[m][g]])
                    S.barrier()
            S.barrier()
            if stop_after == "A5":
                S.finish()
                return nc

            with ExitStack() as s6:
                wo = sb(s6, "wo", [128, 8, D], BF16)
                R_wo = Reg()
                stg6 = [sb(s6, f"stg6{i}", [128, 8, 256], F32) for i in range(2)]
                R_stg6 = [Reg(), Reg()]
                woutv = wout_d.rearrange("(k p) n -> p k n", p=128)
                for blk in range(4):
                    load_w(stg6, R_stg6, wo[:, :, blk * 256:(blk + 1) * 256], [(0, woutv[:, :, blk * 256:(blk + 1) * 256], 256)], R_wo, 256)
                g1bc = sb(s6, "g1bc", [128, D], F32)
                A2bc = sb(s6, "A2bc", [128, D], F32)
                sh2bc = sb(s6, "sh2bc", [128, D], F32)
                R_bc = Reg()
                S.dma("sp", g1bc[:], modrows_d[s:s + 1, 2 * D:3 * D].to_broadcast([128, D]), reads=[R_modrows], writes=[R_bc], nowaw=True)
                S.dma("sp", sh2bc[:], modrows_d[s:s + 1, 3 * D:4 * D].to_broadcast([128, D]), reads=[R_modrows], writes=[R_bc], nowaw=True)
                S.dma("sp", A2bc[:], modrows_d[s:s + 1, 4 * D:5 * D].to_broadcast([128, D]), reads=[R_modrows], writes=[R_bc], nowaw=True)
                n2bc = sb(s6, "n2bc", [128, D], F32)
                S.dma("sp", n2bc[:], n2g_d.to_broadcast([128, D]), writes=[R_bc], nowaw=True)
                stt(A2bc[:], A2bc[:], 1.0, n2bc[:], ALU.add, ALU.mult, [R_bc], [R_bc])
                Mg = [sb(s6, f"Mg{i}", [128, 8, 512], BF16) for i in range(2)]
                R_Mg = [Reg(), Reg()]
                pso6 = [ps(s6, f"pso6{i}", [128, D]) for i in range(2)]
                R_pso6 = [Reg(), Reg()]
                pst6 = ps(s6, "pst6", [128, 8, 128])
                R_pst6 = Reg()
                psl = ps(s6, "psl", [128, 512])
                R_psl = Reg()
                xr = [sb(s6, f"xr{i}", [128, D], F32) for i in range(2)]
                R_xr = [Reg(), Reg()]
                x1t = [sb(s6, f"x1t{i}", [128, D], F32) for i in range(2)]
                R_x1t = [Reg(), Reg()]
                tmp6_ = [sb(s6, f"tmp6{i}", [128, D], F32) for i in range(2)]
                R_tmp6_ = [Reg(), Reg()]
                hxf_ = [sb(s6, f"hxf{i}", [128, D], F32) for i in range(2)]
                R_hxf_ = [Reg(), Reg()]
                hxb = [sb(s6, f"hxb{i}", [128, D], BF16) for i in range(2)]
                R_hxb = [Reg(), Reg()]
                hxT_ = [sb(s6, f"hxT{i}", [128, 8, 128], F32) for i in range(2)]
                R_hxT_ = [Reg(), Reg()]
                junk6_ = [sb(s6, f"junk6{i}", [128, D], F32) for i in range(2)]
                R_junk6_ = [Reg(), Reg()]
                stat6 = [sb(s6, f"stat6{i}", [128, 2], F32) for i in range(2)]
                R_st6 = [Reg(), Reg()]
                lg = sb(s6, "lg", [64, 512], F32)
                R_lg = Reg()
                r0 = 0 if s == 0 else 32
                tmpB_ = [sb(s6, f"tmpB{i}", [128, D], F32) for i in range(2)]
                R_tmpB_ = [Reg(), Reg()]
                psl2 = ps(s6, "psl2", [128, 512])
                psl_ = [psl, psl2]
                R_psl_ = [R_psl, Reg()]

                def a6_A(t):
                    g, tt_ = divmod(t, 4)
                    b = t % 2
                    Mt, RM = Mg[g % 2], R_Mg[g % 2]
                    if tt_ == 0:
                        S.dma("sp", Mt[:], md_d[s, :, :, g * 512:(g + 1) * 512].rearrange("m p t -> p m t"),
                              reads=[R_md[m][g] for m in range(8)], writes=[RM])
                    tmp6, R_tmp6, junk6, R_junk6 = tmp6_[b], R_tmp6_[b], junk6_[b], R_junk6_[b]
                    S.dma("sp", xr[b][:], x_d[s, t * 128:(t + 1) * 128, :], writes=[R_xr[b]])
                    for nb in range(2):
                        for k in range(8):
                            mm(pso6[b][:, nb * 512:(nb + 1) * 512], Mt[:, k, tt_ * 128:(tt_ + 1) * 128], wo[:, k, nb * 512:(nb + 1) * 512],
                               k == 0, k == 7, [RM, R_wo], [R_pso6[b]])
                    tt("dve", tmp6[:], pso6[b][:], g1bc[:], ALU.mult, [R_pso6[b], R_bc], [R_tmp6])
                    tt("dve", x1t[b][:], tmp6[:], xr[b][:], ALU.add, [R_tmp6, R_xr[b]], [R_x1t[b]])
                    S.dma(STQ, x1_d[s, t * 128:(t + 1) * 128, :], x1t[b][:], reads=[R_x1t[b]], writes=[R_x1d], nowaw=True)
                    rms_rstd("a6", x1t[b][:], R_x1t[b], junk6[:], R_junk6, stat6[b][:, 0:1], stat6[b][:, 1:2], R_st6[b], recip=False)

                def a6_B(t):
                    g, tt_ = divmod(t, 4)
                    b = t % 2
                    tmpB, R_tmpB, hxf, R_hxf, hxT, R_hxT = tmpB_[b], R_tmpB_[b], hxf_[b], R_hxf_[b], hxT_[b], R_hxT_[b]
                    pl, Rpl = psl_[g % 2], R_psl_[g % 2]
                    S.op("dve", lambda e: e.reciprocal(out=stat6[b][:, 1:2], in_=stat6[b][:, 0:1]), [R_st6[b]], [R_st6[b]])
                    stt(tmpB[:], x1t[b][:], stat6[b][:, 1:2], A2bc[:], ALU.mult, ALU.mult, [R_x1t[b], R_st6[b], R_bc], [R_tmpB])
                    tt("dve", hxf[:], tmpB[:], sh2bc[:], ALU.add, [R_tmpB, R_bc], [R_hxf])
                    cp("act", hxb[b][:], hxf[:], [R_hxf], [R_hxb[b]])
                    S.dma(STQ, hx2_d[s, t * 128:(t + 1) * 128, :], hxb[b][:], reads=[R_hxb[b]], writes=[R_hx2d], nowaw=True)

                def a6_T(t):
                    b = t % 2
                    hxf, R_hxf, hxT, R_hxT = hxf_[b], R_hxf_[b], hxT_[b], R_hxT_[b]
                    for k in range(8):
                        tr(pst6[:, k, :], hxf[:, k * 128:(k + 1) * 128], identf[:], [R_hxf, R_c], [R_pst6])
                    cp("dve", hxT[:], pst6[:], [R_pst6], [R_hxT])

                def a6_C(t):
                    g, tt_ = divmod(t, 4)
                    b = t % 2
                    hxT, R_hxT = hxT_[b], R_hxT_[b]
                    pl, Rpl = psl_[g % 2], R_psl_[g % 2]
                    for k in range(8):
                        mm(pl[:, tt_ * 128:(tt_ + 1) * 128], wr_f[:, k, :], hxT[:, k, :], k == 0, k == 7, [R_c, R_hxT], [Rpl])
                    if tt_ == 3:
                        act(lg[r0:r0 + 16, :], pl[r0:r0 + 16, :], AF.Exp, [Rpl], [R_lg])
                        mm(pl[r0:r0 + 16, :], ones16[r0:r0 + 16, r0:r0 + 16], lg[r0:r0 + 16, :], True, True, [R_lg, R_c], [Rpl])
                        S.op("dve", lambda e, g=g: e.reciprocal(out=aff[r0:r0 + 16, g * 512:(g + 1) * 512], in_=pl[r0:r0 + 16, :]), [Rpl], [R_aff])
                        tt("dve", aff[r0:r0 + 16, g * 512:(g + 1) * 512], aff[r0:r0 + 16, g * 512:(g + 1) * 512], lg[r0:r0 + 16, :], ALU.mult,
                           [R_aff, R_lg], [R_aff])

                pipeline(16, [a6_A, a6_B, a6_T, a6_C])
                S.barrier()
            S.barrier()
            if stop_after == "A6" and s == 0:
                dbg_dump("aff", aff[:], [R_aff])
                S.barrier()
                S.finish()
                return nc

        postok = sb(top, "postok", [128, 16, 64], F32)
        gatehl = sb(top, "gatehl", [128, 16, 64, 2], BF16)
        R_pt = Reg()
        with ExitStack() as sr:
            work = sb(sr, "work", [64, T], F32)
            mx = sb(sr, "mx", [64, 8], F32)
            mask = sb(sr, "mask", [64, T], F32)
            posm = sb(sr, "posm", [64, T], F32)
            gate = sb(sr, "gate", [64, T], F32)
            posb = sb(sr, "posb", [64, T], BF16)
            gtok = sb(sr, "gtok", [128, 16, 64], F32)
            glo = sb(sr, "glo", [128, 16, 64], F32)
            psT = [ps(sr, f"psT{i}", [128, 8, 64]) for i in range(4)]
            R_w, R_mx, R_mask, R_posm, R_gate, R_psT = Reg(), Reg(), Reg(), Reg(), Reg(), [Reg() for _ in range(4)]
            cp("dve", work[:], aff[:], [R_aff], [R_w])
            for r in range(1 if skip_pre else CAP // 8):
                S.op("dve", lambda e: e.max(out=mx[:], in_=work[:]), [R_w], [R_mx])
                if r < CAP // 8 - 1:
                    S.op("dve", lambda e: e.match_replace(out=work[:], in_to_replace=mx[:], in_values=work[:], imm_value=-1.0), [R_w, R_mx], [R_w])
            ts("dve", mask[:], aff[:], mx[:, 7:8], None, ALU.is_ge, None, [R_aff, R_mx], [R_mask])
            S.op("dve", lambda e: e.memset(work[:], 1.0), [R_mx], [R_w])
            S.op("dve", lambda e: e.tensor_tensor_scan(out=posm[:], data0=work[:], data1=mask[:], initial=0.0, op0=ALU.mult, op1=ALU.add),
                 [R_mask, R_w], [R_posm])
            tt("dve", posm[:], posm[:], mask[:], ALU.mult, [R_posm, R_mask], [R_posm])
            ts("dve", posm[:], posm[:], -1.0, None, ALU.add, None, [R_posm], [R_posm])
            tt("dve", gate[:], aff[:], mask[:], ALU.mult, [R_aff, R_mask], [R_gate])
            cp("dve", posb[:], posm[:], [R_posm], [R_posm])
            R_posr = Reg()
            for s in range(NS):
                S.dma("sp", posr_d[s], posb[s * 32:s * 32 + 16, :], reads=[R_posm], writes=[R_posr], nowaw=True)
            for half in range(2):
                for which, src, dst in ((0, posm, postok), (1, gate, gtok)):
                    p = psT[half * 2 + which]
                    Rp = R_psT[half * 2 + which]
                    for tl in range(8):
                        t = half * 8 + tl
                        tr(p[:, tl, :], src[:, t * 128:(t + 1) * 128], identf[0:64, 0:64], [R_posm, R_gate, R_c], [Rp])
                    cp("act" if which else "dve", dst[:, half * 8:(half + 1) * 8, :], p[:], [Rp], [R_pt])
            cp("dve", gatehl[:, :, :, 0], gtok[:], [R_pt], [R_pt])
            tt("dve", glo[:], gtok[:], gatehl[:, :, :, 0], ALU.subtract, [R_pt], [R_pt])
            cp("dve", gatehl[:, :, :, 1], glo[:], [R_pt], [R_pt])
            if dbg:
                dbg_dump("posm", posm[:], [R_posm])
                dbg_dump("gate", gate[:], [R_gate])
            S.barrier()
        S.barrier()
        if stop_after == "R":
            S.finish()
            return nc

        with ExitStack() as sm:
            NW = 6
            wslot = [sb(sm, f"wslot{i}", [128, 8, D], BF16) for i in range(NW)]
            R_ws = [Reg() for _ in range(NW)]
            stgm = [sb(sm, f"stgm{i}", [128, 2, D], F32) for i in range(2)]
            R_stgm = [Reg() for _ in range(2)]
            mcast = [0]
            tokhl = sb(sm, "tokhl", [128, 16, 2], BF16)
            R_tok = Reg()
            S.dma("sp", tokhl[:], tokhl_d, writes=[R_tok])
            gt4 = sb(sm, "gt4", [128, 16, 64, 4], BF16)
            R_gt4 = Reg()
            cp("dve", gt4[:, :, :, 0:2], gatehl[:], [R_pt], [R_gt4])
            for r_ in range(64):
                S.op("dve", lambda en, r_=r_: en.tensor_copy(out=gt4[:, :, r_, 2:4], in_=tokhl[:]), [R_tok], [R_gt4], nowaw=True)
            St = [sb(sm, f"St{i}", [128, 16, CAP], BF16) for i in range(2)]
            R_St = [Reg(), Reg()]
            xtok = [sb(sm, f"xtok{i}", [128, 2, D], BF16) for i in range(3)]
            R_xtok = [Reg() for _ in range(3)]
            xeT_ = [sb(sm, f"xeT{i}", [128, 8, CAP], BF16) for i in range(2)]
            R_xe_ = [Reg(), Reg()]
            hT = sb(sm, "hT", [128, 8, CAP], BF16)
            R_hT = Reg()
            sa_ = [sb(sm, f"sa{i}", [128, CAP], F32) for i in range(2)]
            R_sa = [Reg(), Reg()]
            gsl_ = [sb(sm, f"gsl{i}", [128, 2, 4], F32) for i in range(3)]
            gs1_ = [sb(sm, f"gs1{i}", [128, 2], F32) for i in range(3)]
            idxf_ = [sb(sm, f"idxf{i}", [128, 2], F32) for i in range(3)]
            idxu_ = [sb(sm, f"idxu{i}", [128, 2], mybir.dt.uint32) for i in range(3)]
            R_gs_ = [Reg() for _ in range(3)]
            yes = [sb(sm, f"yes{i}", [128, D], F32) for i in range(2)]
            R_yes = [Reg(), Reg()]
            g2bc = sb(sm, "g2bcM", [128, NS, D], F32)
            R_g2 = Reg()
            for s_ in range(NS):
                S.dma("sp", g2bc[:, s_, :], modrows_d[s_:s_ + 1, 5 * D:6 * D].to_broadcast([128, D]), reads=[R_modrows], writes=[R_g2], nowaw=True)
            x1_flat = x1_d.rearrange("s t d -> (s t) d")
            R_acc = [Reg() for _ in range(NS)]
            psx = [ps(sm, f"psx{i}", [128, 1024], BF16) for i in range(2)]
            R_psx = [Reg(), Reg()]
            psa = [ps(sm, f"psa{i}", [128, 512]) for i in range(2)]
            R_psa = [Reg(), Reg()]
            psg = ps(sm, "psg", [128, 2, 256])
            R_psg = Reg()
            psy = [ps(sm, f"psy{i}", [128, D]) for i in range(1)]
            R_psy = [Reg()]
            R_ye = Reg()
            wsel = [0]
            wq = []

            pend_cast = [None]

            def load_expert_mat(src_d):
                i = wsel[0] % NW
                wsel[0] += 1
                v = src_d.rearrange("(k p) n -> p k n", p=128)
                tok = {"left": 4}
                for blk in range(4):
                    j = mcast[0]
                    mcast[0] += 1
                    stg_, Rs_ = stgm[j % 2], R_stgm[j % 2]

                    def emit_dma(blk=blk, stg_=stg_, Rs_=Rs_):
                        S.dma("sp", stg_[:], v[:, 2 * blk:2 * blk + 2, :], writes=[Rs_])

                    def emit_cast(blk=blk, stg_=stg_, Rs_=Rs_, j=j):
                        cp(("act", "dve")[(j // 2) % 2], wslot[i][:, 2 * blk:2 * blk + 2, :], stg_[:], [Rs_], [R_ws[i]], nowaw=True)
                        tok["left"] -= 1

                    wq.append(emit_dma)
                    if pend_cast[0] is not None:
                        wq.append(pend_cast[0])
                    pend_cast[0] = emit_cast
                return wslot[i], R_ws[i], tok

            def drain(n=1):
                for _ in range(n):
                    if wq:
                        wq.pop(0)()

            def ensure(tok):
                while tok["left"] > 0:
                    if wq:
                        wq.pop(0)()
                    else:
                        pend_cast[0]()
                        pend_cast[0] = None

            units = [(e, s) for e in range(NE) for s in range(NS)]
            NU = len(units)
            hx2_flat = hx2_d.rearrange("s t d -> (s t) d")

            def m_P0(u):
                e, s = units[u]
                row = s * 32 + e
                Sx, RS = St[u % 2], R_St[u % 2]
                ops = []
                for t in range(16):
                    ops.append(lambda t=t: S.op("dve", lambda en: en.tensor_scalar(out=Sx[:, t, :], in0=iota[:], scalar1=postok[:, t, row:row + 1],
                                                                                  scalar2=None, op0=ALU.is_equal), [R_pt, R_c], [RS], nowaw=True))
                return ops

            def m_P1(u):
                e, s = units[u]
                row = s * 32 + e
                Sx, RS = St[u % 2], R_St[u % 2]
                k3 = u % 3
                for half in range(2):
                    for t in range(16):
                        mm(psg[:, half, 0:4], Sx[:, t, half * 128:(half + 1) * 128], gt4[:, t, row, :], t == 0, t == 15, [RS, R_gt4], [R_psg])
                cp("dve", gsl_[k3][:], psg[:, :, 0:4], [R_psg], [R_gs_[k3]])
                tt("dve", gs1_[k3][:], gsl_[k3][:, :, 0], gsl_[k3][:, :, 1], ALU.add, [R_gs_[k3]], [R_gs_[k3]])
                stt(idxf_[k3][:], gsl_[k3][:, :, 2], float(s * T), gsl_[k3][:, :, 3], ALU.add, ALU.add, [R_gs_[k3]], [R_gs_[k3]])
                cp("dve", idxu_[k3][:], idxf_[k3][:], [R_gs_[k3]], [R_gs_[k3]])
                for half in range(2):
                    S.dma_custom("pool", lambda en, half=half: en.indirect_dma_start(
                        out=xtok[k3][:, half, :], out_offset=None, in_=hx2_flat,
                        in_offset=bass.IndirectOffsetOnAxis(ap=idxu_[k3][:, half:half + 1], axis=0)),
                        reads=[R_gs_[k3], R_hx2d], writes=[R_xtok[k3]], nowaw=(half == 1))

            def m_P2(u):
                k3 = u % 3
                xe, Rxe = xeT_[u % 2], R_xe_[u % 2]
                for half in range(2):
                    for dk in range(8):
                        tr(psx[half][:, dk * 128:(dk + 1) * 128], xtok[k3][:, half, dk * 128:(dk + 1) * 128], identb[:],
                           [R_xtok[k3], R_c], [R_psx[half]])
                    S.op("act", lambda en, half=half: en.activation(out=xe[:, :, half * 128:(half + 1) * 128],
                                                                   in_=psx[half][:].rearrange("p (k j) -> p k j", k=8), func=AF.Copy),
                         [R_psx[half]], [Rxe], nowaw=(half == 1))

            n_a = [0]
            n_y = [0]
            Wcur = {}

            def m_Fgu(u, fillers):
                e, s = units[u]
                xeT, R_xe = xeT_[u % 2], R_xe_[u % 2]
                if s == 0:
                    Wcur["g"], Wcur["u"], Wcur["d"] = Wn["g"], Wn["u"], Wn["d"]
                    if e + 1 < NE:
                        Wn["g"] = load_expert_mat(weg_d[e + 1])
                        Wn["u"] = load_expert_mat(weu_d[e + 1])
                        Wn["d"] = load_expert_mat(wed_d[e + 1])
                Wg, RWg, tokg = Wcur["g"]
                Wu, RWu, toku = Wcur["u"]
                Wd, RWd, tokd = Wcur["d"]
                ensure(tokg)
                ensure(toku)
                for mc in range(8):
                    i = n_a[0] % 2
                    n_a[0] += 1
                    for k in range(8):
                        mm(psa[i][:, 0:256], Wg[:, k, mc * 128:(mc + 1) * 128], xeT[:, k, :], k == 0, k == 7, [RWg, R_xe], [R_psa[i]])
                    for k in range(8):
                        mm(psa[i][:, 256:512], Wu[:, k, mc * 128:(mc + 1) * 128], xeT[:, k, :], k == 0, k == 7, [RWu, R_xe], [R_psa[i]])
                    act(sa_[i][:], psa[i][:, 0:256], AF.Silu, [R_psa[i]], [R_sa[i]])
                    tt("dve", hT[:, mc, :], psa[i][:, 256:512], sa_[i][:], ALU.mult, [R_psa[i], R_sa[i]], [R_hT])
                    for _ in range(2):
                        if fillers:
                            fillers.pop(0)()
                    if mc % 2 == 1:
                        drain(2)

            def m_Fdn(u):
                e, s = units[u]
                k3 = u % 3
                gs1, R_gs = gs1_[k3], R_gs_[k3]
                Wd, RWd, tokd = Wcur["d"]
                ensure(tokd)
                for half in range(2):
                    yb = yes[n_y[0] % 2]
                    Ry = R_yes[n_y[0] % 2]
                    n_y[0] += 1
                    for nb in range(2):
                        if half == 0:
                            dst, Rd = psy[0][:, nb * 512:(nb + 1) * 512], R_psy[0]
                        else:
                            dst, Rd = psa[nb][:], R_psa[nb]
                        for k in range(8):
                            mm(dst, hT[:, k, half * 128:(half + 1) * 128], Wd[:, k, nb * 512:(nb + 1) * 512], k == 0, k == 7, [R_hT, RWd], [Rd])
                    if half == 0:
                        act(yb[:], psy[0][:], AF.Copy, [R_psy[0], R_gs], [Ry], scale=gs1[:, half:half + 1])
                    else:
                        for nb in range(2):
                            S.op("act", lambda en, nb=nb: en.activation(out=yb[:, nb * 512:(nb + 1) * 512], in_=psa[nb][:], func=AF.Copy, scale=gs1[:, half:half + 1]),
                                 [R_psa[nb], R_gs], [Ry], nowaw=(nb == 1))
                    tt("dve", yb[:], yb[:], g2bc[:, s, :], ALU.mult, [Ry, R_g2], [Ry])
                    S.dma_custom("pool", lambda en, half=half, yb=yb: en.indirect_dma_start(
                        out=x1_flat, out_offset=bass.IndirectOffsetOnAxis(ap=idxu_[k3][:, half:half + 1], axis=0),
                        in_=yb[:], in_offset=None, compute_op=ALU.add),
                        reads=[Ry, R_gs, R_x1d], writes=[R_acc[s]], nowaw=(half == 1))
                    drain(2)

            Wn = {"g": load_expert_mat(weg_d[0]), "u": load_expert_mat(weu_d[0]), "d": load_expert_mat(wed_d[0])}
            drain(len(wq))
            for step in range(NU + 3):
                fillers = m_P0(step) if step < NU else []
                if 0 <= step - 3 < NU:
                    m_Fgu(step - 3, fillers)
                while fillers:
                    fillers.pop(0)()
                if 0 <= step - 1 < NU:
                    m_P1(step - 1)
                if 0 <= step - 2 < NU:
                    m_P2(step - 2)
                if 0 <= step - 3 < NU:
                    m_Fdn(step - 3)
            drain(len(wq))
            S.barrier()
        S.barrier()

        if stop_after == "M":
            S.finish()
            return nc

        with ExitStack() as sc:
            fgbc = sb(sc, "fgbc", [128, D], F32)
            R_bc2 = Reg()
            S.dma("sp", fgbc[:], fng_d.to_broadcast([128, D]), writes=[R_bc2])
            x1r = [sb(sc, f"x1r{i}", [128, D], F32) for i in range(3)]
            R_x1r = [Reg(), Reg(), Reg()]
            junkc = [sb(sc, f"junkc{i}", [128, D], F32) for i in range(2)]
            R_junkc = [Reg(), Reg()]
            statc = [sb(sc, f"statc{i}", [128, 2], F32) for i in range(3)]
            R_stc = [Reg(), Reg(), Reg()]
            ot = [sb(sc, f"ot{i}", [128, D], F32) for i in range(3)]
            R_ot = [Reg(), Reg(), Reg()]
            R_out = Reg()
            tiles = [(s, t) for s in range(NS) for t in range(16)]

            def c_L(i):
                s, t = tiles[i]
                k = i % 3
                S.dma("sp", x1r[k][:], x1_d[s, t * 128:(t + 1) * 128, :], reads=[R_x1d, R_acc[s]], writes=[R_x1r[k]])
                rms_rstd("c", x1r[k][:], R_x1r[k], junkc[i % 2][:], R_junkc[i % 2], statc[k][:, 0:1], statc[k][:, 1:2], R_stc[k], recip=False)

            def c_T(i):
                s, t = tiles[i]
                k = i % 3
                S.op("dve", lambda e: e.reciprocal(out=statc[k][:, 1:2], in_=statc[k][:, 0:1]), [R_stc[k]], [R_stc[k]])
                stt(ot[k][:], x1r[k][:], statc[k][:, 1:2], fgbc[:], ALU.mult, ALU.mult, [R_x1r[k], R_stc[k], R_bc2], [R_ot[k]])
                S.dma(STQ, out_d[s, t * 128:(t + 1) * 128, :], ot[k][:], reads=[R_ot[k]], writes=[R_out], nowaw=True)

            pipeline(len(tiles), [c_L, c_T])
            S.barrier()
        S.barrier()
        S.finish()
        if os.environ.get("MARKS"):
            for ln, c in S.marks:
                print("MARK line", ln, c)
        print("program: insts", S.n_inst, "waits", S.n_wait, {k: v for k, v in S.cnt.items()})
    return nc


def _consts():
    f32 = np.float32
    rows = T // 64
    row = np.repeat(np.arange(rows, dtype=f32), 64)
    col = np.tile(np.arange(64, dtype=f32), rows)
    n_freq = 16
    inv_freq = (np.float32(10000.0) ** (-np.arange(n_freq, dtype=f32) / np.float32(n_freq))).astype(f32)
    ang_r = row[:, None] * inv_freq[None, :]
    ang_c = col[:, None] * inv_freq[None, :]
    ang = np.concatenate([ang_r, ang_r, ang_c, ang_c], axis=-1).astype(f32)
    cos = np.cos(ang).astype(f32).T
    sin = np.sin(ang).astype(f32).T
    cosT = np.ascontiguousarray(np.concatenate([cos, cos], axis=0))
    sinT = np.ascontiguousarray(np.concatenate([sin, sin], axis=0))
    rm = np.zeros((128, 128), f32)
    for m in range(128):
        if (m % 32) < 16:
            rm[m + 16, m] = -1.0
        else:
            rm[m - 16, m] = 1.0
    p = np.arange(128)[:, None]
    r = np.arange(128)[None, :]
    bf = ml_dtypes.bfloat16
    return {
        "cosT": cosT, "sinT": sinT, "rmat": rm.astype(bf),
        "mask_le": np.where(p <= r, 0.0, -30000.0).astype(f32).astype(bf), "mask_ge": np.where(p >= r, 0.0, -30000.0).astype(f32).astype(bf),
        "ident_bf": np.eye(128, dtype=f32).astype(bf), "ident_f": np.eye(128, dtype=f32),
        "iota256": np.ascontiguousarray(np.broadcast_to(np.arange(256, dtype=f32)[None, :], (128, 256))),
        "iota_p": np.stack([np.arange(128, dtype=f32), np.arange(128, dtype=f32) + 128], axis=1),
        "tokhl": np.stack([np.broadcast_to(np.arange(128, dtype=f32)[:, None], (128, 16)),
                           np.broadcast_to(128.0 * np.arange(16, dtype=f32)[None, :], (128, 16))], axis=2).astype(bf),
    }


def make_in_maps(inputs):
    f = lambda a: np.ascontiguousarray(np.asarray(a, dtype=np.float32))
    x, c, ctx, c_ctx = f(inputs["x"]), f(inputs["c"]), f(inputs["ctx"]), f(inputs["c_ctx"])
    b_mod = f(inputs["b_mod"])[0]
    shared = {
        "w_mod": f(inputs["w_mod"])[0], "b_mod": b_mod[None, :],
        "bmodT": np.ascontiguousarray(b_mod[:2048].reshape(16, 128).T),
        "n1gT": np.ascontiguousarray(f(inputs["norm1_g"])[0].reshape(8, 128).T),
        "n2g": f(inputs["norm2_g"]), "fng": f(inputs["final_norm_g"])[None, :],
        "w_in": f(inputs["w_in"])[0], "sink": f(inputs["attn_sink"])[0].reshape(1, 16),
        "convwT": np.ascontiguousarray(f(inputs["conv_w"])[0].reshape(3, 8, 128).transpose(2, 1, 0)),
        "w_pa": f(inputs["w_proj_attn"])[0], "w_pc": f(inputs["w_proj_conv"])[0], "w_out": f(inputs["w_out"])[0],
        "w_r": f(inputs["w_router"])[0], "w_eg": f(inputs["w_exp_gate"])[0], "w_eu": f(inputs["w_exp_up"])[0],
        "w_ed": f(inputs["w_exp_down"])[0],
    }
    shared.update(_consts())
    maps = []
    for i in range(8):
        vecs = np.stack([c[2 * i], c[2 * i + 1], c_ctx, np.zeros_like(c_ctx)], axis=1)
        m = dict(shared)
        m["x"] = x[2 * i:2 * i + 2]
        m["ctx"] = ctx[2 * i:2 * i + 2]
        m["cvec"] = np.ascontiguousarray(vecs.reshape(8, 128, 4).transpose(1, 0, 2))
        maps.append(m)
    return maps


def kernel(**inputs):
    maps = make_in_maps(inputs)
    nc = build_program()
    res = run_bass_kernel_spmd(nc, maps, core_ids=list(range(8)))
    return np.concatenate([r["out"] for r in res.results], axis=0).astype(np.float32)
```
